# Optimizing a Trainium2 kernel written in Bass

```python
import jax, jax.numpy as jnp
from jax import lax
import numpy as np

D_MODEL = 1024
BATCH = 2
SEQ = 8192
DEPTH = 1
DEC_BATCH = 16
DEC_SEQ = 64
PAST_LEN = 4096

CHUNK = 64
LEFT_CHUNKS = 8
BAND_CHUNKS = LEFT_CHUNKS + 1
WINDOW_ROWS = LEFT_CHUNKS * CHUNK
HEAD_DIM = 64
N_HEADS_A = 8
N_HEADS_B = 8
WIDTH_A = N_HEADS_A * HEAD_DIM
WIDTH_B = N_HEADS_B * HEAD_DIM
MAX_REL = 256
Q_BLOCK = 128
N_GROUPS = 4
EXPERTS_PER_GROUP = 8
N_EXPERTS = N_GROUPS * EXPERTS_PER_GROUP
TOP_K = 2
D_EXPERT = 128
IN_COLS = 3 * WIDTH_A + 3 * WIDTH_B + N_HEADS_B + 2 * D_MODEL
EPS = 1e-6
NEG = -1e30

kernel_name = "hybrid_chunkband_fox_hiermoe_step"


def rmsnorm(x, g):
    xf = x.astype(jnp.float32)
    y = xf * lax.rsqrt(jnp.mean(xf * xf, axis=-1, keepdims=True) + EPS)
    return (y * g.astype(jnp.float32)).astype(x.dtype)


def mixer_inputs(h, w_in, b_f, q_norm_a, k_norm_a, q_norm_b, k_norm_b):
    n, t, _ = h.shape
    proj = h @ w_in
    sizes = [WIDTH_A, WIDTH_A, WIDTH_A, WIDTH_B, WIDTH_B, WIDTH_B, N_HEADS_B, 2 * D_MODEL]
    points = [int(p) for p in np.cumsum(sizes)[:-1]]
    q_a, k_a, v_a, q_b, k_b, v_b, f_logit, gate_logit = jnp.split(proj, points, axis=-1)
    q_a = rmsnorm(q_a.reshape(n, t, N_HEADS_A, HEAD_DIM), q_norm_a)
    k_a = rmsnorm(k_a.reshape(n, t, N_HEADS_A, HEAD_DIM), k_norm_a)
    v_a = v_a.reshape(n, t, N_HEADS_A, HEAD_DIM)
    q_b = rmsnorm(q_b.reshape(n, t, N_HEADS_B, HEAD_DIM), q_norm_b)
    k_b = rmsnorm(k_b.reshape(n, t, N_HEADS_B, HEAD_DIM), k_norm_b)
    v_b = v_b.reshape(n, t, N_HEADS_B, HEAD_DIM)
    logf = jax.nn.log_sigmoid((f_logit + b_f).astype(jnp.float32))
    return q_a, k_a, v_a, q_b, k_b, v_b, logf, gate_logit


def rel_bias_block(rel_bias, n_q, n_k, offset):
    dist = jnp.arange(n_q)[:, None] + offset - jnp.arange(n_k)[None, :]
    idx = jnp.clip(dist, -MAX_REL, MAX_REL) + MAX_REL
    return rel_bias[:, idx].astype(jnp.float32)


def band_attention_prompt(q, k, v, rel_bias):
    n, s, h, d = q.shape
    nc = s // CHUNK
    qc = q.reshape(n, nc, CHUNK, h, d)
    pad = ((0, 0), (LEFT_CHUNKS, 0), (0, 0), (0, 0), (0, 0))
    kp = jnp.pad(k.reshape(n, nc, CHUNK, h, d), pad)
    vp = jnp.pad(v.reshape(n, nc, CHUNK, h, d), pad)
    band_idx = jnp.arange(nc)[:, None] + jnp.arange(BAND_CHUNKS)[None, :]
    kb = kp[:, band_idx].reshape(n, nc, BAND_CHUNKS * CHUNK, h, d)
    vb = vp[:, band_idx].reshape(n, nc, BAND_CHUNKS * CHUNK, h, d)
    key_chunk = jnp.arange(nc)[:, None] - LEFT_CHUNKS + jnp.arange(BAND_CHUNKS * CHUNK)[None, :] // CHUNK
    valid = key_chunk >= 0
    bias = rel_bias_block(rel_bias, CHUNK, BAND_CHUNKS * CHUNK, WINDOW_ROWS)
    sc = jnp.einsum('bnqhd,bnkhd->bnhqk', qc, kb).astype(jnp.float32) * (HEAD_DIM ** -0.5) + bias[None, None]
    sc = jnp.where(valid[None, :, None, None, :], sc, NEG)
    p = jax.nn.softmax(sc, axis=-1).astype(v.dtype)
    o = jnp.einsum('bnhqk,bnkhd->bnqhd', p, vb)
    return o.reshape(n, s, h * d)


def band_attention_sample(q, k, v, k_cache, v_cache, rel_bias):
    n, t, h, d = q.shape
    p_rows = k_cache.shape[1]
    kk = jnp.concatenate([k_cache.astype(k.dtype), k], axis=1)
    vv = jnp.concatenate([v_cache.astype(v.dtype), v], axis=1)
    bias = rel_bias_block(rel_bias, t, p_rows + t, p_rows)
    sc = jnp.einsum('bqhd,bkhd->bhqk', q, kk).astype(jnp.float32) * (HEAD_DIM ** -0.5) + bias[None]
    p = jax.nn.softmax(sc, axis=-1).astype(v.dtype)
    o = jnp.einsum('bhqk,bkhd->bqhd', p, vv)
    return o.reshape(n, t, h * d), kk[:, -p_rows:], vv[:, -p_rows:]


def fox_prompt(q, k, v, logf):
    n, s, h, d = q.shape
    nqb = s // Q_BLOCK
    c = jnp.cumsum(logf, axis=1)
    c_k = jnp.transpose(c, (0, 2, 1))
    k_pos = jnp.arange(s)
    qb = jnp.moveaxis(q.reshape(n, nqb, Q_BLOCK, h, d), 1, 0)
    cb = jnp.moveaxis(c.reshape(n, nqb, Q_BLOCK, h), 1, 0)

    def block(args):
        q_i, c_i, i = args
        q_pos = i * Q_BLOCK + jnp.arange(Q_BLOCK)
        decay = jnp.transpose(c_i, (0, 2, 1))[..., None] - c_k[:, :, None, :]
        sc = jnp.einsum('bqhd,bkhd->bhqk', q_i, k).astype(jnp.float32) * (HEAD_DIM ** -0.5) + decay
        sc = jnp.where(k_pos[None, :] <= q_pos[:, None], sc, NEG)
        p = jax.nn.softmax(sc, axis=-1).astype(v.dtype)
        return jnp.einsum('bhqk,bkhd->bqhd', p, v)

    o = lax.map(block, (qb, cb, jnp.arange(nqb)))
    return jnp.moveaxis(o, 0, 1).reshape(n, s, h * d)


def fox_sample(q, k, v, logf, k_cache, v_cache, logf_cache):
    n, t, h, d = q.shape
    p_rows = k_cache.shape[1]
    kk = jnp.concatenate([k_cache.astype(k.dtype), k], axis=1)
    vv = jnp.concatenate([v_cache.astype(v.dtype), v], axis=1)
    lf = jnp.concatenate([logf_cache.astype(jnp.float32), logf], axis=1)
    c_k = jnp.transpose(jnp.cumsum(lf, axis=1), (0, 2, 1))
    c_q = c_k[:, :, p_rows:]
    sc = jnp.einsum('bqhd,bkhd->bhqk', q, kk).astype(jnp.float32) * (HEAD_DIM ** -0.5)
    sc = sc + c_q[..., None] - c_k[:, :, None, :]
    mask = jnp.arange(p_rows + t)[None, :] <= p_rows + jnp.arange(t)[:, None]
    sc = jnp.where(mask, sc, NEG)
    p = jax.nn.softmax(sc, axis=-1).astype(v.dtype)
    o = jnp.einsum('bhqk,bkhd->bqhd', p, vv)
    return o.reshape(n, t, h * d)


def merge_branches(o_a, o_b, gate_logit, w_pa, w_pb, w_o):
    g_a, g_b = jnp.split(jax.nn.sigmoid(gate_logit), 2, axis=-1)
    return (g_a * (o_a @ w_pa) + g_b * (o_b @ w_pb)) @ w_o


def mixer_prompt(x, g_mix, w_in, b_f, q_norm_a, k_norm_a, q_norm_b, k_norm_b, rel_bias, w_pa, w_pb, w_o):
    h = rmsnorm(x, g_mix)
    q_a, k_a, v_a, q_b, k_b, v_b, logf, gate_logit = mixer_inputs(h, w_in, b_f, q_norm_a, k_norm_a, q_norm_b, k_norm_b)
    o_a = band_attention_prompt(q_a, k_a, v_a, rel_bias)
    o_b = fox_prompt(q_b, k_b, v_b, logf)
    y = x + merge_branches(o_a, o_b, gate_logit, w_pa, w_pb, w_o)
    rows = min(WINDOW_ROWS, x.shape[1])
    return y, (k_a[:, -rows:], v_a[:, -rows:], k_b, v_b, logf)


def mixer_sample(x, c_a_k, c_a_v, c_b_k, c_b_v, c_b_logf, g_mix, w_in, b_f, q_norm_a, k_norm_a, q_norm_b, k_norm_b, rel_bias, w_pa, w_pb, w_o):
    h = rmsnorm(x, g_mix)
    q_a, k_a, v_a, q_b, k_b, v_b, logf, gate_logit = mixer_inputs(h, w_in, b_f, q_norm_a, k_norm_a, q_norm_b, k_norm_b)
    o_a, new_a_k, new_a_v = band_attention_sample(q_a, k_a, v_a, c_a_k, c_a_v, rel_bias)
    o_b = fox_sample(q_b, k_b, v_b, logf, c_b_k, c_b_v, c_b_logf)
    y = x + merge_branches(o_a, o_b, gate_logit, w_pa, w_pb, w_o)
    return y, (new_a_k, new_a_v, k_b, v_b, logf)


def hier_moe(x, g_ffn, w_rg, b_rg, w_re, b_re, w1, w3, w2):
    n, t, d = x.shape
    hx = rmsnorm(x, g_ffn).reshape(n * t, d)
    coarse = (hx @ w_rg).astype(jnp.float32) + b_rg.astype(jnp.float32)
    p_g = jax.nn.softmax(coarse, axis=-1)
    grp = jnp.argmax(coarse, axis=-1)
    pg_sel = jnp.take_along_axis(p_g, grp[:, None], axis=-1)
    fine = jnp.einsum('td,gde->tge', hx, w_re).astype(jnp.float32) + b_re.astype(jnp.float32)
    fine_sel = jnp.take_along_axis(fine, grp[:, None, None], axis=1)[:, 0]
    top_p, top_i = lax.top_k(jax.nn.softmax(fine_sel, axis=-1), TOP_K)
    top_p = top_p / jnp.sum(top_p, axis=-1, keepdims=True)
    expert = grp[:, None] * EXPERTS_PER_GROUP + top_i
    combine = jnp.sum(jax.nn.one_hot(expert, N_EXPERTS, dtype=jnp.float32) * (pg_sel * top_p)[..., None], axis=1)
    hid = jax.nn.silu(jnp.einsum('td,edf->tef', hx, w1)) * jnp.einsum('td,edf->tef', hx, w3)
    hid = hid * combine.astype(hid.dtype)[..., None]
    y = jnp.einsum('tef,efd->td', hid, w2)
    return y.reshape(n, t, d)


def setup_inputs(seed: int = 0) -> dict:
    key = jax.random.key(seed)
    ks = jax.random.split(key, 32)
    f32 = jnp.float32
    nrm = lambda k, shape, scale: jax.random.normal(k, shape, f32) * scale
    a_rows = min(WINDOW_ROWS, PAST_LEN)
    return {
        'x_prompt': nrm(ks[0], (BATCH, SEQ, D_MODEL), 1.0),
        'x_sample': nrm(ks[1], (DEC_BATCH, DEC_SEQ, D_MODEL), 1.0),
        'cache_a_k': nrm(ks[2], (DEPTH, DEC_BATCH, a_rows, N_HEADS_A, HEAD_DIM), 1.0),
        'cache_a_v': nrm(ks[3], (DEPTH, DEC_BATCH, a_rows, N_HEADS_A, HEAD_DIM), 1.0),
        'cache_b_k': nrm(ks[4], (DEPTH, DEC_BATCH, PAST_LEN, N_HEADS_B, HEAD_DIM), 1.0),
        'cache_b_v': nrm(ks[5], (DEPTH, DEC_BATCH, PAST_LEN, N_HEADS_B, HEAD_DIM), 1.0),
        'cache_b_logf': jax.nn.log_sigmoid(3.0 + nrm(ks[6], (DEPTH, DEC_BATCH, PAST_LEN, N_HEADS_B), 1.0)),
        'g_mix': 1.0 + nrm(ks[7], (DEPTH, D_MODEL), 0.02),
        'w_in': nrm(ks[8], (DEPTH, D_MODEL, IN_COLS), D_MODEL ** -0.5),
        'b_f': 2.0 + 2.0 * jax.random.uniform(ks[9], (DEPTH, N_HEADS_B), f32),
        'q_norm_a': 1.0 + nrm(ks[10], (DEPTH, HEAD_DIM), 0.02),
        'k_norm_a': 1.0 + nrm(ks[11], (DEPTH, HEAD_DIM), 0.02),
        'q_norm_b': 1.0 + nrm(ks[12], (DEPTH, HEAD_DIM), 0.02),
        'k_norm_b': 1.0 + nrm(ks[13], (DEPTH, HEAD_DIM), 0.02),
        'rel_bias': nrm(ks[14], (DEPTH, N_HEADS_A, 2 * MAX_REL + 1), 0.2),
        'w_pa': nrm(ks[15], (DEPTH, WIDTH_A, D_MODEL), WIDTH_A ** -0.5),
        'w_pb': nrm(ks[16], (DEPTH, WIDTH_B, D_MODEL), WIDTH_B ** -0.5),
        'w_o': nrm(ks[17], (DEPTH, D_MODEL, D_MODEL), D_MODEL ** -0.5),
        'g_ffn': 1.0 + nrm(ks[18], (DEPTH, D_MODEL), 0.02),
        'w_rg': nrm(ks[19], (DEPTH, D_MODEL, N_GROUPS), D_MODEL ** -0.5),
        'b_rg': nrm(ks[20], (DEPTH, N_GROUPS), 0.01),
        'w_re': nrm(ks[21], (DEPTH, N_GROUPS, D_MODEL, EXPERTS_PER_GROUP), D_MODEL ** -0.5),
        'b_re': nrm(ks[22], (DEPTH, N_GROUPS, EXPERTS_PER_GROUP), 0.01),
        'w1': nrm(ks[23], (DEPTH, N_EXPERTS, D_MODEL, D_EXPERT), D_MODEL ** -0.5),
        'w3': nrm(ks[24], (DEPTH, N_EXPERTS, D_MODEL, D_EXPERT), D_MODEL ** -0.5),
        'w2': nrm(ks[25], (DEPTH, N_EXPERTS, D_EXPERT, D_MODEL), D_EXPERT ** -0.5),
    }


def reference(x_prompt, x_sample, cache_a_k, cache_a_v, cache_b_k, cache_b_v, cache_b_logf,
              g_mix, w_in, b_f, q_norm_a, k_norm_a, q_norm_b, k_norm_b, rel_bias, w_pa, w_pb, w_o,
              g_ffn, w_rg, b_rg, w_re, b_re, w1, w3, w2):
    x_p, x_s = x_prompt, x_sample
    st_p, st_s = [], []
    for l in range(DEPTH):
        mix = (g_mix[l], w_in[l], b_f[l], q_norm_a[l], k_norm_a[l], q_norm_b[l], k_norm_b[l],
               rel_bias[l], w_pa[l], w_pb[l], w_o[l])
        ffn = (g_ffn[l], w_rg[l], b_rg[l], w_re[l], b_re[l], w1[l], w3[l], w2[l])
        x_p, sp = mixer_prompt(x_p, *mix)
        x_p = x_p + hier_moe(x_p, *ffn)
        x_s, ss = mixer_sample(x_s, cache_a_k[l], cache_a_v[l], cache_b_k[l], cache_b_v[l], cache_b_logf[l], *mix)
        x_s = x_s + hier_moe(x_s, *ffn)
        st_p.append(sp)
        st_s.append(ss)

    def stack(states, i):
        return jnp.stack([s[i] for s in states])

    return (x_p, x_s,
            stack(st_p, 0), stack(st_p, 1), stack(st_p, 2), stack(st_p, 3), stack(st_p, 4),
            stack(st_s, 0), stack(st_s, 1), stack(st_s, 2), stack(st_s, 3), stack(st_s, 4))
```

```python
import contextlib
import os
import numpy as np
import concourse.bass as bass
import concourse.mybir as mybir
from concourse.bass_utils import run_bass_kernel_spmd

F32 = mybir.dt.float32
BF16 = mybir.dt.bfloat16
AF = mybir.ActivationFunctionType
ALU = mybir.AluOpType
AX = mybir.AxisListType

D = 1024
SEQ = 8192
NH = 8
HD = 64
EPS = 1e-6
IN_COLS = 5128
C_QA, C_KA, C_VA, C_QB, C_KB, C_VB, C_F, C_G = 0, 512, 1024, 1536, 2048, 2560, 3072, 3080

ENGS = ["tensor", "scalar", "vector", "gpsimd", "sync"]
EPOCH = 4000


class Buf:
    __slots__ = ("name", "writers", "readers")

    def __init__(self, name=""):
        self.name = name
        self.writers = []
        self.readers = []


class Prog:
    def __init__(self, nc, same_engine_wait=True, n_dma_sems=8):
        self.nc = nc
        self.same = same_engine_wait
        self.streams = {e: [] for e in ENGS}
        self.count = {e: 0 for e in ENGS}
        self.epoch = {e: 0 for e in ENGS}
        self.sems = {}
        self.known = {e: {} for e in ENGS}
        self.n_dma_sems = n_dma_sems
        self.dma_rr = {e: 0 for e in ENGS}
        self.dma_cnt = {}
        self.dma_ep = {}
        self._ctx = []

    def _sem(self, key):
        if key not in self.sems:
            cm = self.nc.semaphore("s_" + "_".join(str(k) for k in key))
            h = cm.__enter__()
            self._ctx.append(cm)
            self.sems[key] = h
        return self.sems[key]

    def _waits(self, eng, deps):
        need = {}
        for (key, v) in deps:
            if key[0] == eng and len(key) == 2 and (eng == "tensor" or not self.same):
                continue
            if self.known[eng].get(key, 0) >= v:
                continue
            if need.get(key, 0) < v:
                need[key] = v
        out = []
        for key, v in need.items():
            self.known[eng][key] = v
            out.append((self._sem(key), v))
        return out

    @staticmethod
    def _deps(reads, writes):
        deps = []
        for b in reads:
            deps += b.writers
        for b in writes:
            deps += b.writers
            deps += b.readers
        return deps

    @staticmethod
    def _commit(ev, reads, writes):
        for b in writes:
            b.writers = [ev]
            b.readers = []
        for b in reads:
            if b not in writes:
                b.readers = [r for r in b.readers if r[0] != ev[0]] + [ev]

    def op(self, eng, fn, reads=(), writes=()):
        deps = self._deps(reads, writes)
        waits = self._waits(eng, deps)
        if self.count[eng] >= EPOCH:
            self.epoch[eng] += 1
            self.count[eng] = 0
        key = (eng, self.epoch[eng])
        sem = self._sem(key)
        self.count[eng] += 1
        ev = (key, self.count[eng])
        self.streams[eng].append((waits, fn, sem, 1))
        self._commit(ev, reads, writes)
        return ev

    def dma(self, eng, fn, reads=(), writes=()):
        deps = self._deps(reads, writes)
        i = self.dma_rr[eng]
        self.dma_rr[eng] = (i + 1) % self.n_dma_sems
        ep = self.dma_ep.get((eng, i), 0)
        key = (eng, "d", i, ep)
        prev = self.dma_cnt.get(key, 0)
        if prev:
            deps = list(deps) + [(key, prev)]
        if prev >= EPOCH:
            ep += 1
            self.dma_ep[(eng, i)] = ep
            key = (eng, "d", i, ep)
            prev = 0
        waits = self._waits(eng, deps)
        sem = self._sem(key)
        self.dma_cnt[key] = prev + 16
        ev = (key, prev + 16)
        self.streams[eng].append((waits, fn, sem, 16))
        self._commit(ev, reads, writes)
        return ev

    def barrier(self):
        evs = []
        for e in ENGS:
            if self.count[e] > 0:
                evs.append(((e, self.epoch[e]), self.count[e]))
        for key, v in self.dma_cnt.items():
            evs.append((key, v))
        for e in ENGS:
            waits = self._waits(e, [ev for ev in evs if not (ev[0][0] == e and len(ev[0]) == 2)])
            if waits:
                self.streams[e].append((waits, None, None, 0))

    def finish(self):
        nc = self.nc
        final = [(self._sem(key), v) for key, v in self.dma_cnt.items()]
        streams = self.streams

        def run(e, name):
            for (waits, fn, sem, inc) in streams[name]:
                for (s, v) in waits:
                    e.wait_ge(s, v)
                if fn is not None:
                    fn(e).then_inc(sem, inc)
            if name == "sync":
                for (s, v) in final:
                    e.wait_ge(s, v)

        with nc.Block() as block:
            @block.sync
            def _(e):
                run(e, "sync")

            @block.tensor
            def _(e):
                run(e, "tensor")

            @block.scalar
            def _(e):
                run(e, "scalar")

            @block.vector
            def _(e):
                run(e, "vector")

            @block.gpsimd
            def _(e):
                run(e, "gpsimd")

    def close(self):
        for cm in reversed(self._ctx):
            cm.__exit__(None, None, None)
        self._ctx = []


class T:
    def __init__(self, ap, name):
        self.ap = ap
        self.b = Buf(name)

    def __getitem__(self, k):
        return self.ap[k]

    def v(self, pattern, **kw):
        return self.ap.rearrange(pattern, **kw)


class Arena:
    def __init__(self, ap2d):
        self.ap = ap2d
        self.n = ap2d.shape[1]
        self.top = 0

    def alloc(self, name, cols, dt=F32, parts=128):
        words = cols if dt == F32 else (cols + 1) // 2
        a = self.top
        self.top += words
        assert self.top <= self.n, ("SBUF arena overflow", name, self.top, self.n)
        v = self.ap[:, a:a + words]
        if dt != F32:
            v = v.bitcast(dt)[:, 0:cols]
        if parts < 128:
            v = v[0:parts]
        return T(v, name)


NTOK = 2176
SARENA = 50500


class K:
    pass


def build_program():
    nc = bass.Bass("TRN2", target_bir_lowering=False)
    k = K()
    k.nc = nc

    def din(name, shape):
        return nc.dram_tensor(name, list(shape), F32, kind="ExternalInput").ap()

    def dout(name, shape):
        return nc.dram_tensor(name, list(shape), F32, kind="ExternalOutput").ap()

    I = k.I = {}
    for name, shape in [
        ("xfull", [SEQ, D]), ("xown", [2048, D]), ("xhalo", [2048, D]), ("xs", [128, D]),
        ("cak", [2, 512, 512]), ("cav", [2, 512, 512]), ("cbk", [2, 4096, 512]), ("cbv", [2, 4096, 512]),
        ("cblf", [2, 4096, NH]),
        ("g_mix", [1, D]), ("w_in", [D, IN_COLS]), ("b_f", [1, NH]),
        ("q_norm_a", [1, HD]), ("k_norm_a", [1, HD]), ("q_norm_b", [1, HD]), ("k_norm_b", [1, HD]),
        ("rel_bias", [NH, 513]), ("w_pa", [512, D]), ("w_pb", [512, D]), ("w_o", [D, D]),
        ("g_ffn", [1, D]), ("w_rg", [D, 4]), ("b_rg", [1, 4]), ("w_re", [4, D, 8]), ("b_re", [1, 32]),
        ("w1", [32, D, 128]), ("w3", [32, D, 128]), ("w2", [32, 128, D]),
        ("wmeta", [1, 256]), ("hvalid", [1, 4]), ("qpos", [1, 512]),
    ]:
        I[name] = din(name, shape)
    O = k.O = {}
    for name, shape in [
        ("o_y", [2048, D]), ("o_ys", [128, D]), ("o_ak", [512, 512]), ("o_av", [512, 512]),
        ("o_bk", [SEQ, 512]), ("o_bv", [SEQ, 512]), ("o_blf", [SEQ, NH]),
        ("o_aks", [2, 512, 512]), ("o_avs", [2, 512, 512]),
        ("o_bks", [128, 512]), ("o_bvs", [128, 512]), ("o_blfs", [128, NH]),
    ] + ([("o_dbg", [NTOK, 1024]), ("o_kt", [NH, 70, 256]), ("o_vs", [256, 520]), ("o_qb", [70, NH * 512])] if os.environ.get("KNOROUTER") else []):
        O[name] = dout(name, shape)
    S = k.S = {}
    S["KT"] = nc.dram_tensor("KTs", [NH, 70, SEQ], BF16).ap()
    S["VS"] = nc.dram_tensor("VSs", [SEQ, NH * 65], BF16).ap()
    S["KTS"] = nc.dram_tensor("KTSs", [2, NH, 70, 4224], BF16).ap()
    S["VSS"] = nc.dram_tensor("VSSs", [2, 4224, NH * 65], BF16).ap()
    S["OT"] = nc.dram_tensor("OTs", [NTOK, 1024], BF16).ap()
    S["Y1"] = nc.dram_tensor("Y1s", [NTOK, D], F32).ap()
    S["EXT"] = nc.dram_tensor("EXTs", [NH, 896], F32).ap()

    P = k.P = Prog(nc)
    with contextlib.ExitStack() as es:
        arena_t = es.enter_context(nc.sbuf_tensor("arena", [128, SARENA], F32))
        psum_t = es.enter_context(nc.psum_tensor("parena", [128, 4096], F32))
        k.A = Arena(arena_t[:, :])
        k.psum = psum_t
        setup_consts(k)
        base = k.A.top
        sel = os.environ.get("KPH", "full,own,merge,moe").split(",")
        for ph in (phase_full, phase_own, phase_merge, phase_moe):
            if ph.__name__[6:] not in sel:
                continue
            k.A.top = base
            k.pbuf = {}
            ph(k)
            P.barrier()
        P.finish()
        P.close()
    return nc


def pbank(k, name, b0, nb=1, dt=F32):
    v = k.psum[:, b0 * 512:(b0 + nb) * 512]
    if dt != F32:
        v = v.bitcast(dt)
    return T(v, name)


def bcast_rows(ap_row, n, off=0):
    return bass.AP(ap_row.tensor, ap_row.offset + off, [[0, 128], [1, n]])


V_, S_, G_, PE_ = "vector", "scalar", "gpsimd", "tensor"


def setup_consts(k):
    P, A, I = k.P, k.A, k.I
    C = k.C = {}
    for name, cols, dt in [("identf", 128, F32), ("ident", 128, BF16), ("U", 128, F32), ("U2", 128, F32), ("Ubf", 128, BF16),
                           ("ones", 128, F32), ("gmix", D, F32), ("gffn", D, F32), ("gqa", 512, F32), ("gka", 512, F32),
                           ("gqb", 512, F32), ("gkb", 512, F32), ("bf", NH, F32), ("wm", 256, F32), ("epsb", 2, F32),
                           ("hv", 4, F32), ("offs", 32, F32), ("carry2", NH, F32), ("brg", 4, F32), ("bre", 32, F32)]:
        C[name] = A.alloc(name, cols, dt)
    identf, ident, U, U2, Ubf, ones = C["identf"], C["ident"], C["U"], C["U2"], C["Ubf"], C["ones"]
    P.op(G_, lambda e: e.memset(identf[:], 0.0), writes=[identf.b])
    P.op(G_, lambda e: e.affine_select(out=identf[:], in_=identf[:], pattern=[[-1, 128]], compare_op=ALU.not_equal,
                                       fill=1.0, base=0, channel_multiplier=1), reads=[identf.b], writes=[identf.b])
    P.op(V_, lambda e: e.tensor_copy(out=ident[:], in_=identf[:]), reads=[identf.b], writes=[ident.b])
    P.op(G_, lambda e: e.memset(ones[:], 1.0), writes=[ones.b])
    P.op(G_, lambda e: e.affine_select(out=U[:], in_=ones[:], pattern=[[1, 128]], compare_op=ALU.is_ge, fill=0.0,
                                       base=0, channel_multiplier=-1), reads=[ones.b], writes=[U.b])
    P.op(V_, lambda e: e.tensor_copy(out=Ubf[:], in_=U[:]), reads=[U.b], writes=[Ubf.b])
    P.op(V_, lambda e: e.tensor_copy(out=U2[:], in_=U[:]), reads=[U.b], writes=[U2.b])
    P.op(V_, lambda e: e.memset(U2[0:64, 64:128], 0.0), reads=[U2.b], writes=[U2.b])
    P.op(G_, lambda e: e.memset(C["epsb"][:, 0:1], float(D * EPS)), writes=[C["epsb"].b])
    P.op(G_, lambda e: e.memset(C["epsb"][:, 1:2], float(HD * EPS)), reads=[C["epsb"].b], writes=[C["epsb"].b])
    for name, src, scale in [("gmix", "g_mix", 32.0), ("gffn", "g_ffn", 32.0)]:
        t = C[name]
        P.dma("sync", lambda e, t=t, src=src: e.dma_start(out=t[:], in_=bcast_rows(I[src], D)), writes=[t.b])
        P.op(V_, lambda e, t=t, scale=scale: e.tensor_scalar(out=t[:], in0=t[:], scalar1=scale, scalar2=None, op0=ALU.mult),
             reads=[t.b], writes=[t.b])
    for name, src, scale in [("gqa", "q_norm_a", 1.0), ("gka", "k_norm_a", 8.0), ("gqb", "q_norm_b", 1.0), ("gkb", "k_norm_b", 8.0)]:
        t = C[name]
        P.dma("sync", lambda e, t=t, src=src: e.dma_start(
            out=t.v("p (h d) -> p h d", d=HD), in_=bass.AP(I[src].tensor, 0, [[0, 128], [0, NH], [1, HD]])), writes=[t.b])
        if scale != 1.0:
            P.op(V_, lambda e, t=t, scale=scale: e.tensor_scalar(out=t[:], in0=t[:], scalar1=scale, scalar2=None, op0=ALU.mult),
                 reads=[t.b], writes=[t.b])
    for name, src, n in [("bf", "b_f", NH), ("wm", "wmeta", 256), ("hv", "hvalid", 4), ("brg", "b_rg", 4), ("bre", "b_re", 32)]:
        t = C[name]
        P.dma("sync", lambda e, t=t, src=src, n=n: e.dma_start(out=t[:], in_=bcast_rows(I[src], n)), writes=[t.b])


def rsqrt_act(k, out, ss, n):
    P, epsb = k.P, k.C["epsb"]
    col = {1024: 0, 64: 1}[n]
    P.op(S_, lambda e: e.activation(out=out[:], in_=ss[:], func=AF.Ln, bias=epsb[:, col:col + 1], scale=1.0),
         reads=[ss.b, epsb.b], writes=[out.b])
    P.op(S_, lambda e: e.activation(out=out[:], in_=out[:], func=AF.Exp, scale=-0.5), reads=[out.b], writes=[out.b])


def get(k, name, cols, dt=F32, parts=128):
    if name not in k.pbuf:
        k.pbuf[name] = k.A.alloc(name, cols, dt, parts)
    return k.pbuf[name]


def gtmp(k, name, cols, dt=F32, parts=128):
    return get(k, name + getattr(k, "tag", ""), cols, dt, parts)


def rms_tile(k, X, gain, tr, tag=""):
    P, C = k.P, k.C
    junk = gtmp(k, "junk", D, BF16)
    ss = gtmp(k, "ss", 1)
    rs = gtmp(k, "rs", 1)
    H = gtmp(k, "hbf", D, BF16)
    HT = get(k, "hT" + tag, D, BF16)
    P.op(S_, lambda e: e.activation(out=junk[:], in_=X[:], func=AF.Square, accum_out=ss[:, 0:1]), reads=[X.b], writes=[junk.b, ss.b])
    rsqrt_act(k, rs, ss, D)
    P.op(V_, lambda e: e.scalar_tensor_tensor(out=H[:], in0=X[:], scalar=rs[:, 0:1], in1=gain[:], op0=ALU.mult, op1=ALU.mult),
         reads=[X.b, rs.b, gain.b], writes=[H.b])
    ident = C["ident"]
    for kc in range(8):
        P.op(PE_, lambda e, kc=kc: e.transpose(tr[:, kc * 128:(kc + 1) * 128], H[:, kc * 128:(kc + 1) * 128], ident[:]),
             reads=[H.b, ident.b], writes=[tr.b])
    P.op(S_, lambda e: e.copy(out=HT[:], in_=tr[:, 0:D]), reads=[tr.b], writes=[HT.b])
    return HT


def proj(k, dst, dst_ap, HT, W, w_ap_fn):
    P = k.P
    for kc in range(8):
        P.op(PE_, lambda e, kc=kc: e.matmul(dst_ap, lhsT=HT[:, kc * 128:(kc + 1) * 128], rhs=w_ap_fn(kc), start=(kc == 0), stop=(kc == 7)),
             reads=[HT.b, W.b], writes=[dst.b])


def headnorm(k, src, src_ap, gain, out):
    P = k.P
    sq = gtmp(k, "sq", 512)
    ssk = gtmp(k, "ssk", NH)
    rstdk = gtmp(k, "rstdk", NH)
    t1 = gtmp(k, "t1", 512)
    P.op(S_, lambda e: e.activation(out=sq[:], in_=src_ap, func=AF.Square), reads=[src.b], writes=[sq.b])
    P.op(V_, lambda e: e.tensor_reduce(out=ssk[:], in_=sq.v("p (h d) -> p h d", d=HD), axis=AX.X, op=ALU.add), reads=[sq.b], writes=[ssk.b])
    rsqrt_act(k, rstdk, ssk, HD)
    P.op(V_, lambda e: e.tensor_tensor(out=t1.v("p (h d) -> p h d", d=HD), in0=src_ap.rearrange("p (h d) -> p h d", d=HD),
                                       in1=rstdk[:].unsqueeze(2).to_broadcast([128, NH, HD]), op=ALU.mult),
         reads=[src.b, rstdk.b], writes=[t1.b])
    P.op(G_, lambda e: e.tensor_tensor(out=out[:], in0=t1[:], in1=gain[:], op=ALU.mult), reads=[t1.b, gain.b], writes=[out.b])


def logf_from(k, fsrc, f_ap, LF):
    P, bf = k.P, k.C["bf"]
    z, za, ze, zl, zm = (gtmp(k, n, NH) for n in ["z", "za", "ze", "zl", "zm"])
    P.op(V_, lambda e: e.tensor_tensor(out=z[:], in0=f_ap, in1=bf[:], op=ALU.add), reads=[fsrc.b, bf.b], writes=[z.b])
    P.op(V_, lambda e: e.scalar_tensor_tensor(out=za[:], in0=z[:], scalar=-1.0, in1=z[:], op0=ALU.mult, op1=ALU.max), reads=[z.b], writes=[za.b])
    P.op(S_, lambda e: e.activation(out=ze[:], in_=za[:], func=AF.Exp, scale=-1.0), reads=[za.b], writes=[ze.b])
    P.op(S_, lambda e: e.activation(out=zl[:], in_=ze[:], func=AF.Ln, bias=1.0), reads=[ze.b], writes=[zl.b])
    P.op(V_, lambda e: e.tensor_single_scalar(out=zm[:], in_=z[:], scalar=0.0, op=ALU.min), reads=[z.b], writes=[zm.b])
    P.op(V_, lambda e: e.tensor_sub(out=LF[:], in0=zm[:], in1=zl[:]), reads=[zm.b, zl.b], writes=[LF.b])


def cumsum_tile(k, LF, Umat, carry, sm, csb, update_carry):
    P, ones = k.P, k.C["ones"]
    P.op(PE_, lambda e: e.matmul(sm[:, 8:16], lhsT=Umat[:], rhs=LF[:], start=True, stop=True), reads=[Umat.b, LF.b], writes=[sm.b])
    if update_carry:
        P.op(PE_, lambda e: e.matmul(sm[:, 16:24], lhsT=ones[:], rhs=LF[:], start=True, stop=True), reads=[ones.b, LF.b], writes=[sm.b])
    P.op(V_, lambda e: e.tensor_tensor(out=csb[:], in0=sm[:, 8:16], in1=carry[:], op=ALU.add), reads=[sm.b, carry.b], writes=[csb.b])
    if update_carry:
        P.op(V_, lambda e: e.tensor_tensor(out=carry[:], in0=sm[:, 16:24], in1=carry[:], op=ALU.add), reads=[sm.b, carry.b], writes=[carry.b])


def split3(k, csb, ST, c0, sign):
    P = k.P
    r1, r2 = gtmp(k, "r1", NH), gtmp(k, "r2", NH)
    v = ST.v("p (h c) -> p h c", c=70)
    P.op(V_, lambda e: e.tensor_scalar(out=v[:, :, c0], in0=csb[:], scalar1=sign, scalar2=None, op0=ALU.mult), reads=[csb.b], writes=[ST.b])
    P.op(V_, lambda e: e.scalar_tensor_tensor(out=r1[:], in0=csb[:], scalar=sign, in1=v[:, :, c0], op0=ALU.mult, op1=ALU.subtract),
         reads=[csb.b, ST.b], writes=[r1.b])
    P.op(V_, lambda e: e.tensor_copy(out=v[:, :, c0 + 1], in_=r1[:]), reads=[r1.b], writes=[ST.b])
    P.op(V_, lambda e: e.tensor_sub(out=r2[:], in0=r1[:], in1=v[:, :, c0 + 1]), reads=[r1.b, ST.b], writes=[r2.b])
    P.op(V_, lambda e: e.tensor_copy(out=v[:, :, c0 + 2], in_=r2[:]), reads=[r2.b], writes=[ST.b])


def aug_transpose(k, ST, ktp, OUT):
    P, ident = k.P, k.C["ident"]
    v = ST.v("p (h c) -> p h c", c=70)
    for h in range(NH):
        P.op(PE_, lambda e, h=h: e.transpose(ktp[0:70, h * 128:(h + 1) * 128], v[:, h, :], ident[:]), reads=[ST.b, ident.b], writes=[ktp.b])
    P.op(S_, lambda e: e.copy(out=OUT[:], in_=ktp[0:70, 0:NH * 128]), reads=[ktp.b], writes=[OUT.b])


def phase_full(k):
    P, C, I, O, S = k.P, k.C, k.I, k.O, k.S
    Wkvf = get(k, "Wkvf", 8 * 1032, BF16)
    Wv = Wkvf.v("p (k c) -> p k c", c=1032)
    for kc in range(8):
        P.dma(G_, lambda e, kc=kc: e.dma_start(out=Wv[:, kc, :], in_=I["w_in"][kc * 128:(kc + 1) * 128, C_KB:C_KB + 1032]), writes=[Wkvf.b])
    tr = pbank(k, "tr", 0, 1, BF16)
    kbp = [pbank(k, "kbp%d" % i, 1 + i) for i in range(2)]
    vbp = [pbank(k, "vbp%d" % i, 3 + i) for i in range(2)]
    sm = pbank(k, "sm", 5)
    offp = pbank(k, "offp", 6)
    ktp = pbank(k, "ktp", 7, 1, BF16)
    xin = [get(k, "xin%d" % i, D) for i in range(2)]
    kout = [get(k, "kout%d" % i, 512) for i in range(2)]
    vout = [get(k, "vout%d" % i, 512) for i in range(2)]
    kst = [get(k, "kst%d" % i, NH * 70, BF16) for i in range(2)]
    vst = [get(k, "vst%d" % i, NH * 65, BF16) for i in range(2)]
    ktT = [get(k, "ktT%d" % i, NH * 128, BF16, parts=70) for i in range(2)]
    lf = [get(k, "lf%d" % i, NH) for i in range(2)]
    lfw2 = [get(k, "lfw%d" % i, 32) for i in range(2)]
    carry = get(k, "carry", NH)
    csb2 = [get(k, "csb%d" % i, NH) for i in range(2)]
    for i in range(2):
        P.op(G_, lambda e, i=i: e.memset(kst[i][:], 1.0), writes=[kst[i].b])
        P.op(G_, lambda e, i=i: e.memset(vst[i][:], 1.0), writes=[vst[i].b])
    cnt = [0]

    lcnt = [0]

    def kv_loads(mode, src, r0):
        i = lcnt[0] % 2
        lcnt[0] += 1
        if mode == "x":
            X = xin[i]
            P.dma("sync", lambda e: e.dma_start(out=X[:], in_=src[r0:r0 + 128, :]), writes=[X.b])
        else:
            ck, cv, cl = src
            KO, VO, LF = kout[i], vout[i], lf[i]
            P.dma("sync", lambda e: e.dma_start(out=KO[:], in_=ck[r0:r0 + 128, :]), writes=[KO.b])
            P.dma("sync", lambda e: e.dma_start(out=VO[:], in_=cv[r0:r0 + 128, :]), writes=[VO.b])
            P.dma("sync", lambda e: e.dma_start(out=LF[:], in_=cl[r0:r0 + 128, :]), writes=[LF.b])

    def kv_tile(mode, src, r0, dst_k, dst_v, dst_lf, kt_dsts, vs_dsts, Umat, carry_t, upd, wm_ft=None, first=False, last=False):
        i = cnt[0] % 2
        cnt[0] += 1
        k.tag = "_p%d" % i
        lfw, csb = lfw2[i], csb2[i]
        KO, VO, KS, VSb, KTT, LF = kout[i], vout[i], kst[i], vst[i], ktT[i], lf[i]
        if mode == "x":
            X = xin[i]
            HT = rms_tile(k, X, C["gmix"], tr, tag=str(i))
            proj(k, kbp[i], kbp[i][:, 0:512], HT, Wkvf, lambda kc: Wv[:, kc, 0:512])
            proj(k, vbp[i], vbp[i][:, 0:512], HT, Wkvf, lambda kc: Wv[:, kc, 512:1024])
            proj(k, sm, sm[:, 0:8], HT, Wkvf, lambda kc: Wv[:, kc, 1024:1032])
            headnorm(k, kbp[i], kbp[i][:, 0:512], C["gkb"], KO)
            P.op(S_, lambda e: e.copy(out=VO[:], in_=vbp[i][:, 0:512]), reads=[vbp[i].b], writes=[VO.b])
            logf_from(k, sm, sm[:, 0:8], LF)
            P.dma("sync", lambda e: e.dma_start(out=dst_k, in_=KO[:]), reads=[KO.b])
            P.dma("sync", lambda e: e.dma_start(out=dst_v, in_=VO[:]), reads=[VO.b])
            P.dma("sync", lambda e: e.dma_start(out=dst_lf, in_=LF[:]), reads=[LF.b])
        P.op(G_, lambda e: e.tensor_copy(out=KS.v("p (h c) -> p h c", c=70)[:, :, 0:HD], in_=KO.v("p (h d) -> p h d", d=HD)),
             reads=[KO.b], writes=[KS.b])
        P.op(G_, lambda e: e.tensor_copy(out=VSb.v("p (h c) -> p h c", c=65)[:, :, 0:HD], in_=VO.v("p (h d) -> p h d", d=HD)),
             reads=[VO.b], writes=[VSb.b])
        P.op(V_, lambda e: e.memset(VSb.v("p (h c) -> p h c", c=65)[:, :, 64:65], 1.0), reads=[VSb.b], writes=[VSb.b])
        P.op(V_, lambda e: e.memset(KS.v("p (h c) -> p h c", c=70)[:, :, 64:67], 1.0), reads=[KS.b], writes=[KS.b])
        for (dst, p0, p1) in vs_dsts:
            P.dma(G_, lambda e, dst=dst, p0=p0, p1=p1: e.dma_start(out=dst, in_=VSb[p0:p1, :]), reads=[VSb.b])
        cumsum_tile(k, LF, Umat, carry_t, sm, csb, upd)
        if wm_ft is not None:
            P.op(V_, lambda e: e.tensor_tensor(out=lfw.v("p (a h) -> p a h", h=NH), in0=LF[:].unsqueeze(1).to_broadcast([128, 4, NH]),
                                               in1=C["wm"][:, wm_ft * 4:(wm_ft + 1) * 4].unsqueeze(2).to_broadcast([128, 4, NH]), op=ALU.mult),
                 reads=[LF.b, C["wm"].b], writes=[lfw.b])
            P.op(PE_, lambda e: e.matmul(offp[:, 0:32], lhsT=C["ones"][:], rhs=lfw[:], start=first, stop=last),
                 reads=[C["ones"].b, lfw.b], writes=[offp.b])
        split3(k, csb, KS, 67, -1.0)
        aug_transpose(k, KS, ktp, KTT)
        KTv = KTT.v("p (h t) -> p h t", t=128)
        for (dst, c0, c1) in kt_dsts:
            P.dma(G_, lambda e, dst=dst, c0=c0, c1=c1: e.dma_start(out=dst, in_=KTv[:, :, c0:c1]), reads=[KTT.b])

    NT = SEQ // 128
    tiles = []
    for ft in range(NT):
        r0 = ft * 128
        tiles.append(dict(pre=("zero" if ft == 0 else None), mode="x", src=I["xfull"], r0=r0,
                          args=(O["o_bk"][r0:r0 + 128, :], O["o_bv"][r0:r0 + 128, :], O["o_blf"][r0:r0 + 128, :],
                                [(S["KT"][:, :, r0:r0 + 128].rearrange("h p t -> p h t"), 0, 128)], [(S["VS"][r0:r0 + 128, :], 0, 128)],
                                C["U"], carry, True), kw=dict(wm_ft=ft, first=(ft == 0), last=(ft == NT - 1)),
                          post=("offs" if ft == NT - 1 else None)))
    for sbi in range(2):
        for ct in range(32):
            r0 = ct * 128
            tiles.append(dict(pre=("zero" if ct == 0 else None), mode="cache", src=(I["cbk"][sbi], I["cbv"][sbi], I["cblf"][sbi]), r0=r0,
                              args=(None, None, None, [(S["KTS"][sbi, :, :, r0:r0 + 128].rearrange("h p t -> p h t"), 0, 128)],
                                    [(S["VSS"][sbi, r0:r0 + 128, :], 0, 128)], C["U"], carry, True), kw={},
                              post=(("c2", sbi) if ct == 31 else None)))
    tiles.append(dict(pre=None, mode="x", src=I["xs"], r0=0,
                      args=(O["o_bks"][:, :], O["o_bvs"][:, :], O["o_blfs"][:, :],
                            [(S["KTS"][sbi, :, :, 4096:4160].rearrange("h p t -> p h t"), sbi * 64, sbi * 64 + 64) for sbi in range(2)],
                            [(S["VSS"][sbi, 4096:4160, :], sbi * 64, sbi * 64 + 64) for sbi in range(2)], C["U2"], C["carry2"], False),
                      kw={}, post=None))
    kv_loads(tiles[0]["mode"], tiles[0]["src"], tiles[0]["r0"])
    for ti, td in enumerate(tiles):
        if ti + 1 < len(tiles):
            nx = tiles[ti + 1]
            kv_loads(nx["mode"], nx["src"], nx["r0"])
        if td["pre"] == "zero":
            P.op(V_, lambda e: e.memset(carry[:], 0.0), reads=[carry.b], writes=[carry.b])
        kv_tile(td["mode"], td["src"], td["r0"], *td["args"], **td["kw"])
        if td["post"] == "offs":
            P.op(V_, lambda e: e.tensor_copy(out=C["offs"][:], in_=offp[:, 0:32]), reads=[offp.b], writes=[C["offs"].b])
        elif td["post"] is not None:
            sbi = td["post"][1]
            P.op(V_, lambda e, sbi=sbi: e.tensor_copy(out=C["carry2"][sbi * 64:(sbi + 1) * 64, :], in_=carry[sbi * 64:(sbi + 1) * 64, :]),
                 reads=[carry.b], writes=[C["carry2"].b])

def slot(s):
    return (s // 6) * 512 + (s % 6) * 80


def phase_own(k):
    k.tag = ""
    P, C, I, O, S, A = k.P, k.C, k.I, k.O, k.S, k.A
    I32 = mybir.dt.int32
    Wq = get(k, "Wq", 8 * 2056, BF16)
    Wv = Wq.v("p (k c) -> p k c", c=2056)
    for kc in range(8):
        P.dma(G_, lambda e, kc=kc: e.dma_start(out=Wv[:, kc, 0:2048], in_=I["w_in"][kc * 128:(kc + 1) * 128, 0:2048]), writes=[Wq.b])
        P.dma(G_, lambda e, kc=kc: e.dma_start(out=Wv[:, kc, 2048:2056], in_=I["w_in"][kc * 128:(kc + 1) * 128, C_F:C_F + 8]), writes=[Wq.b])
    BT = get(k, "BT", NH * 640, BF16)
    BTv = BT.v("p (h c) -> p h c", c=640)
    ext = get(k, "ext", 896, F32, parts=8)
    Xt = get(k, "Xt", 128)
    Jm = get(k, "Jm", 128)
    ps0 = pbank(k, "ps0", 0)
    P.dma("sync", lambda e: e.dma_start(out=ext[:, 0:384], in_=I["rel_bias"][:, 129:513]), writes=[ext.b])
    P.op(V_, lambda e: e.tensor_copy(out=ext[:, 384:896], in_=ext[:, 383:384].to_broadcast([8, 512])), reads=[ext.b], writes=[ext.b])
    P.dma("sync", lambda e: e.dma_start(out=S["EXT"][:, :], in_=ext[:]), reads=[ext.b], writes=[BT.b])
    P.op(G_, lambda e: e.memset(Jm[:], 0.0), writes=[Jm.b])
    P.op(G_, lambda e: e.affine_select(out=Jm[:], in_=Jm[:], pattern=[[1, 128]], compare_op=ALU.not_equal, fill=1.0,
                                       base=-127, channel_multiplier=1), reads=[Jm.b], writes=[Jm.b])
    for h in range(NH):
        for t in range(5):
            P.dma("sync", lambda e, h=h, t=t: e.dma_start(out=Xt[:], in_=bass.AP(S["EXT"].tensor, h * 896 + 512 - 128 * t, [[1, 128], [1, 128]])),
                  reads=[BT.b], writes=[Xt.b])
            P.op(PE_, lambda e: e.matmul(ps0[:, 0:128], lhsT=Jm[:], rhs=Xt[:], start=True, stop=True), reads=[Jm.b, Xt.b], writes=[ps0.b])
            P.op(V_, lambda e, h=h, t=t: e.tensor_copy(out=BTv[:, h, t * 128:(t + 1) * 128], in_=ps0[:, 0:128]), reads=[ps0.b], writes=[BT.b])
    P.op(V_, lambda e: e.memset(BTv[0:64, :, 64:128], -30000.0), reads=[BT.b], writes=[BT.b])
    P.op(V_, lambda e: e.memset(BTv[64:128, :, 512:576], -30000.0), reads=[BT.b], writes=[BT.b])
    M = get(k, "M", 16 * 512, BF16)
    Mv = M.v("p (c t) -> p c t", t=512)
    qrow = get(k, "qrow", 512)
    kpi = T(get(k, "kpi", 16).ap.bitcast(I32), "kpi")
    kpf = get(k, "kpf", 16)
    P.dma("sync", lambda e: e.dma_start(out=qrow[:], in_=bcast_rows(I["qpos"], 512)), writes=[qrow.b])
    P.op(G_, lambda e: e.iota(kpi[:], pattern=[[128, 16]], base=0, channel_multiplier=1), writes=[kpi.b])
    P.op(V_, lambda e: e.tensor_copy(out=kpf[:], in_=kpi[:]), reads=[kpi.b], writes=[kpf.b])
    for c in range(16):
        P.op(V_, lambda e, c=c: e.tensor_scalar(out=Mv[:, c, :], in0=qrow[:], scalar1=kpf[:, c:c + 1], scalar2=None, op0=ALU.is_ge),
             reads=[qrow.b, kpf.b], writes=[M.b])
        P.op(V_, lambda e, c=c: e.tensor_scalar(out=Mv[:, c, :], in0=Mv[:, c, :], scalar1=30000.0, scalar2=-30000.0, op0=ALU.mult, op1=ALU.add),
             reads=[M.b], writes=[M.b])
    Uadd = get(k, "Uadd", 128, BF16)
    P.op(V_, lambda e: e.tensor_scalar(out=Uadd[:], in0=C["U"][:], scalar1=30000.0, scalar2=-30000.0, op0=ALU.mult, op1=ALU.add),
         reads=[C["U"].b], writes=[Uadd.b])
    Sm = [get(k, "Sm%d" % i, 512) for i in range(2)]

    qaT = get(k, "qaT", 4 * 512, BF16)
    kaT = get(k, "kaT", 4 * 1024, BF16)
    va = [get(k, "va%d" % i, NH * 65, BF16) for i in range(8)]
    qbT = get(k, "qbT", NH * 512, BF16, parts=70)
    qst = get(k, "qst", NH * 70, BF16)
    stag = get(k, "stag", 512, BF16)
    xin = [get(k, "xin%d" % i, D) for i in range(2)]
    fo = [get(k, "fo%d" % i, 512) for i in range(3)]
    lf = get(k, "lf", NH)
    csb = get(k, "csb", NH)
    carryq = get(k, "carryq", NH)
    ost = [get(k, "ost%d" % i, 1024, BF16) for i in range(4)]
    Sb = get(k, "Sb", 640)
    Pb = get(k, "Pb", 640, BF16)
    Pt = [get(k, "Pt%d" % i, 512, BF16) for i in range(2)]
    ktc = [get(k, "ktc%d" % i, NH * 512, BF16, parts=70) for i in range(2)]
    vc = [get(k, "vc%d" % i, 4 * 520, BF16) for i in range(2)]
    rc = get(k, "rc", 1)
    zb = get(k, "zb", 512, BF16)
    P.op(G_, lambda e: e.memset(zb[:], 0.0), writes=[zb.b])
    qaTv = qaT.v("p (g t) -> p g t", t=512)
    kaTv = kaT.v("p (g t) -> p g t", t=1024)
    qbTv = qbT.v("p (h t) -> p h t", t=512)
    P.op(G_, lambda e: e.memset(qst[:], 1.0), writes=[qst.b])
    xcnt = [0]

    def psum_proj():
        return dict(tr=pbank(k, "tr", 0, 1, BF16), pj=[pbank(k, "pj%d" % i, 1 + i) for i in range(2)], sm=pbank(k, "sm", 3),
                    ktp=pbank(k, "ktp", 4, 1, BF16), n=[0])

    def next_pj(pp):
        pp["n"][0] += 1
        return pp["pj"][pp["n"][0] % 2]

    def load_x(src, r0):
        X = xin[xcnt[0] % 2]
        xcnt[0] += 1
        P.dma("sync", lambda e: e.dma_start(out=X[:], in_=src[r0:r0 + 128, :]), writes=[X.b])
        return X

    def to_T4(pp, src_f32, dstv, c0, ncols=128, src_c0=0):
        P.op(G_, lambda e: e.tensor_copy(out=stag[:], in_=src_f32[:]), reads=[src_f32.b], writes=[stag.b])
        tr = pp["tr"]
        for g in range(4):
            P.op(PE_, lambda e, g=g: e.transpose(tr[:, g * 128:(g + 1) * 128], stag[:, g * 128:(g + 1) * 128], C["ident"][:]),
                 reads=[stag.b, C["ident"].b], writes=[tr.b])
        return tr

    def ka_va(pp, HT, kt_idx, dst, vcol_fn, out_k=None, out_v=None):
        pj = next_pj(pp)
        proj(k, pj, pj[:, 0:512], HT, Wq, lambda kc: Wv[:, kc, C_KA:C_KA + 512])
        KAO = fo[0]
        headnorm(k, pj, pj[:, 0:512], C["gka"], KAO)
        if out_k is not None:
            P.dma("sync", lambda e: e.dma_start(out=out_k, in_=KAO[:]), reads=[KAO.b])
        tr = to_T4(pp, KAO, None, 0)
        P.op(S_, lambda e: e.copy(out=dst[0][:, :, dst[1]:dst[1] + 128], in_=tr[:, 0:512].rearrange("p (g t) -> p g t", t=128)),
             reads=[tr.b], writes=[dst[2].b])
        pj2 = next_pj(pp)
        proj(k, pj2, pj2[:, 0:512], HT, Wq, lambda kc: Wv[:, kc, C_VA:C_VA + 512])
        VAO = fo[1]
        P.op(S_, lambda e: e.copy(out=VAO[:], in_=pj2[:, 0:512]), reads=[pj2.b], writes=[VAO.b])
        if out_v is not None:
            P.dma("sync", lambda e: e.dma_start(out=out_v, in_=VAO[:]), reads=[VAO.b])
        if kt_idx is not None:
            VA = va[kt_idx]
            vav = VA.v("p (h c) -> p h c", c=65)
            P.op(G_, lambda e: e.tensor_copy(out=vav[:, :, 0:HD], in_=VAO.v("p (h d) -> p h d", d=HD)), reads=[VAO.b], writes=[VA.b])
            vcol_fn(VA, vav)
        return KAO, VAO

    def ones_col(VA, vav):
        P.op(G_, lambda e: e.memset(vav[:, :, 64:65], 1.0), reads=[VA.b], writes=[VA.b])

    def q_side(pp, HT, Umat, carry_t, upd, qa_dst, qb_dst):
        pj = next_pj(pp)
        proj(k, pj, pj[:, 0:512], HT, Wq, lambda kc: Wv[:, kc, C_QA:C_QA + 512])
        QAO = fo[2]
        headnorm(k, pj, pj[:, 0:512], C["gqa"], QAO)
        tr = to_T4(pp, QAO, None, 0)
        P.op(S_, lambda e: e.copy(out=qa_dst[0], in_=tr[:, 0:512].rearrange("p (g t) -> p g t", t=128)), reads=[tr.b], writes=[qa_dst[1].b])
        pj2 = next_pj(pp)
        proj(k, pj2, pj2[:, 0:512], HT, Wq, lambda kc: Wv[:, kc, C_QB:C_QB + 512])
        QBO = fo[2]
        headnorm(k, pj2, pj2[:, 0:512], C["gqb"], QBO)
        P.op(G_, lambda e: e.tensor_copy(out=qst.v("p (h c) -> p h c", c=70)[:, :, 0:HD], in_=QBO.v("p (h d) -> p h d", d=HD)),
             reads=[QBO.b], writes=[qst.b])
        sm = pp["sm"]
        proj(k, sm, sm[:, 0:8], HT, Wq, lambda kc: Wv[:, kc, 2048:2056])
        logf_from(k, sm, sm[:, 0:8], lf)
        cumsum_tile(k, lf, Umat, carry_t, sm, csb, upd)
        split3(k, csb, qst, 64, 1.0)
        ktp = pp["ktp"]
        qv = qst.v("p (h c) -> p h c", c=70)
        for h in range(NH):
            P.op(PE_, lambda e, h=h: e.transpose(ktp[0:70, h * 128:(h + 1) * 128], qv[:, h, :], C["ident"][:]),
                 reads=[qst.b, C["ident"].b], writes=[ktp.b])
        P.op(S_, lambda e: e.copy(out=qb_dst[0], in_=ktp[0:70, 0:1024].rearrange("p (h t) -> p h t", t=128)), reads=[ktp.b], writes=[qb_dst[1].b])

    def band(nq, q_ap_fn, key_tiles, ost_t, last_rows):
        Sbp = [pbank(k, "Sbp%d" % i, 2 * i, 2) for i in range(2)]
        Obp = [pbank(k, "Obp%d" % i, 4 + 2 * i, 2) for i in range(2)]
        return Sbp, Obp

    def band_unit(Sbp, Obp, u, nq, h, q_ap, k_aps, v_tiles, ost_t, last_rows):
        sp, op_ = Sbp[u % 2], Obp[(u // 8) % 2]
        nk = len(k_aps)
        for t in range(nk):
            rows = last_rows if t == nk - 1 else 128
            P.op(PE_, lambda e, t=t, rows=rows: e.matmul(sp[0:rows, t * 128:t * 128 + nq], lhsT=k_aps[t][0], rhs=q_ap, start=True, stop=True),
                 reads=[k_aps[t][1].b, qaT.b], writes=[sp.b])
        w = (nk - 1) * 128 + nq
        BTh = BTv[:, h, :]
        if nq == 128 and last_rows == 128:
            P.op(V_, lambda e: e.tensor_tensor(out=Sb[:, 0:640], in0=sp[:, 0:640], in1=BTh, op=ALU.add), reads=[sp.b, BT.b], writes=[Sb.b])
            P.op(S_, lambda e: e.activation(out=Pb[:, 0:640], in_=Sb[:, 0:640], func=AF.Exp), reads=[Sb.b], writes=[Pb.b])
        else:
            for t in range(nk):
                rows = last_rows if t == nk - 1 else 128
                P.op(V_, lambda e, t=t, rows=rows: e.tensor_tensor(out=Sb[0:rows, t * 128:t * 128 + nq], in0=sp[0:rows, t * 128:t * 128 + nq],
                                                                   in1=BTv[0:rows, h, t * 128:t * 128 + nq], op=ALU.add),
                     reads=[sp.b, BT.b], writes=[Sb.b])
                P.op(S_, lambda e, t=t, rows=rows: e.activation(out=Pb[0:rows, t * 128:t * 128 + nq], in_=Sb[0:rows, t * 128:t * 128 + nq], func=AF.Exp),
                     reads=[Sb.b], writes=[Pb.b])
        o0 = slot(h)
        for t in range(nk):
            rows = last_rows if t == nk - 1 else 128
            VA = v_tiles[t]
            P.op(PE_, lambda e, t=t, rows=rows, VA=VA: e.matmul(op_[0:nq, o0:o0 + 65], lhsT=Pb[0:rows, t * 128:t * 128 + nq],
                                                               rhs=VA.v("p (h c) -> p h c", c=65)[0:rows, h, :], start=(t == 0), stop=(t == nk - 1)),
                 reads=[Pb.b, VA.b], writes=[op_.b])
        P.op(V_, lambda e: e.reciprocal(out=rc[0:nq, :], in_=op_[0:nq, o0 + 64:o0 + 65]), reads=[op_.b], writes=[rc.b])
        P.op(V_, lambda e: e.tensor_scalar(out=ost_t[0:nq, h * 64:(h + 1) * 64], in0=op_[0:nq, o0:o0 + 64], scalar1=rc[0:nq, 0:1], scalar2=None, op0=ALU.mult),
             reads=[op_.b, rc.b], writes=[ost_t.b])

    def fox(nq, q_ap_fn, chunks, ost_list):
        Sp = [pbank(k, "Sp%d" % i, i) for i in range(2)]
        Op = pbank(k, "Op", 2, 6)
        nqs = (nq + 127) // 128
        qn = min(nq, 128)
        units = []

        def load(ci):
            (kt_src, v_src, nkeys, mask_fn) = chunks[ci]
            KC, VC = ktc[ci % 2], vc[ci % 2]
            KCv = KC.v("p (h t) -> p h t", t=512)
            VCv = VC.v("p (a c) -> p a c", c=520)
            P.dma("sync", lambda e: e.dma_start(out=KCv[:, :, 0:nkeys], in_=kt_src), writes=[KC.b])
            ntile = (nkeys + 127) // 128
            if nkeys >= 128:
                P.dma("sync", lambda e: e.dma_start(out=VCv[:, 0:ntile, :], in_=v_src.rearrange("(a p) c -> p a c", p=128)), writes=[VC.b])
            else:
                P.dma("sync", lambda e: e.dma_start(out=VCv[0:nkeys, 0, :], in_=v_src), writes=[VC.b])

        for ci, (kt_src, v_src, nkeys, mask_fn) in enumerate(chunks):
            KC, VC = ktc[ci % 2], vc[ci % 2]
            KCv = KC.v("p (h t) -> p h t", t=512)
            VCv = VC.v("p (a c) -> p a c", c=520)
            ntile = (nkeys + 127) // 128
            for kt in range(ntile):
                rows = min(128, nkeys - kt * 128)
                for h in range(NH):
                    units.append((ci, kt, h, rows, KC, KCv, VC, VCv, mask_fn(kt) if mask_fn else None))
        load(0)
        for bk in range(6):
            P.op(PE_, lambda e, bk=bk: e.matmul(Op[:, bk * 512:(bk + 1) * 512], lhsT=zb[:, 0:128], rhs=zb[:, 0:512], start=True, stop=True),
                 reads=[zb.b], writes=[Op.b])
        nu = len(units)
        first = {}
        last = {}
        for ui, u in enumerate(units):
            first.setdefault(u[2], ui)
            last[u[2]] = ui

        def pv(ui):
            (ci, kt, h, rows, KC, KCv, VC, VCv, mk) = units[ui]
            PT = Pt[ui % 2]
            for qs in range(nqs):
                o0 = slot(h * nqs + qs)
                P.op(PE_, lambda e, qs=qs, o0=o0: e.matmul(Op[0:qn, o0:o0 + 65], lhsT=PT[0:rows, qs * 128:qs * 128 + qn], rhs=VCv[0:rows, kt, h * 65:(h + 1) * 65],
                                                           start=False, stop=(ui == last[h])), reads=[PT.b, VC.b], writes=[Op.b])

        for ui, (ci, kt, h, rows, KC, KCv, VC, VCv, mk) in enumerate(units):
            SP, PT = Sp[ui % 2], Pt[ui % 2]
            P.op(PE_, lambda e, SP=SP, KCv=KCv, rows=rows, kt=kt, h=h, mk=mk: e.matmul(SP[0:rows, 0:nq], lhsT=KCv[:, h, kt * 128:kt * 128 + rows], rhs=q_ap_fn(h),
                                                                                      start=True, stop=(mk is None)), reads=[KC.b, qbT.b], writes=[SP.b])
            if mk is not None:
                P.op(PE_, lambda e, SP=SP, rows=rows, mk=mk: e.matmul(SP[0:rows, 0:nq], lhsT=C["ident"][0:rows, 0:rows], rhs=mk[0][0:rows, 0:nq],
                                                                      start=False, stop=True), reads=[C["ident"].b, mk[1].b], writes=[SP.b])
            P.op(S_, lambda e, SP=SP, PT=PT, rows=rows: e.activation(out=PT[0:rows, 0:nq], in_=SP[0:rows, 0:nq], func=AF.Exp), reads=[SP.b], writes=[PT.b])
            if ui >= 1:
                pv(ui - 1)
            if (ui == 0 or units[ui - 1][0] != ci) and ci + 1 < len(chunks):
                load(ci + 1)
        pv(nu - 1)
        for h in range(NH):
            for qs in range(nqs):
                o0 = slot(h * nqs + qs)
                ot = ost_list[qs]
                P.op(V_, lambda e, o0=o0: e.reciprocal(out=rc[0:qn, :], in_=Op[0:qn, o0 + 64:o0 + 65]), reads=[Op.b], writes=[rc.b])
                P.op(V_, lambda e, o0=o0, ot=ot, h=h: e.tensor_scalar(out=ot[0:qn, 512 + h * 64:512 + (h + 1) * 64], in0=Op[0:qn, o0:o0 + 64],
                                                                      scalar1=rc[0:qn, 0:1], scalar2=None, op0=ALU.mult), reads=[Op.b, rc.b], writes=[ot.b])

    for n in range(4):
        pp = psum_proj()
        for t in range(4):
            X = load_x(I["xhalo"], n * 512 + t * 128)
            HT = rms_tile(k, X, C["gmix"], pp["tr"])

            def hv_col(VA, vav, n=n):
                P.op(V_, lambda e: e.tensor_copy(out=vav[:, :, 64:65], in_=C["hv"][:, n:n + 1].unsqueeze(1).to_broadcast([128, NH, 1])),
                     reads=[VA.b, C["hv"].b], writes=[VA.b])
            ka_va(pp, HT, t, (kaTv, t * 128, kaT), hv_col)
        P.op(V_, lambda e, n=n: e.tensor_copy(out=carryq[:], in_=C["offs"][:, n * 8:(n + 1) * 8]), reads=[C["offs"].b, carryq.b], writes=[carryq.b])
        for t in range(4):
            r0 = n * 512 + t * 128
            X = load_x(I["xown"], r0)
            HT = rms_tile(k, X, C["gmix"], pp["tr"])
            ka_va(pp, HT, 4 + t, (kaTv, (4 + t) * 128, kaT), ones_col,
                  out_k=(O["o_ak"][t * 128:(t + 1) * 128, :] if n == 3 else None), out_v=(O["o_av"][t * 128:(t + 1) * 128, :] if n == 3 else None))
            q_side(pp, HT, C["U"], carryq, True, (qaTv[:, :, t * 128:(t + 1) * 128], qaT), (qbTv[:, :, t * 128:(t + 1) * 128], qbT))
        if n == 0 and os.environ.get("KNOROUTER"):
            P.dma(G_, lambda e: e.dma_start(out=O["o_qb"][:, :], in_=qbT[:]), reads=[qbT.b])
            P.dma(G_, lambda e: e.dma_start(out=O["o_kt"][:, :, :], in_=S["KT"][:, :, 0:256]))
            P.dma(G_, lambda e: e.dma_start(out=O["o_vs"][:, :], in_=S["VS"][0:256, :]))
        P.barrier()
        Sbp, Obp = band(0, None, None, None, 0)
        u = 0
        for cp in range(4):
            for h in range(NH):
                g, r0 = h // 2, (h % 2) * 64
                k_aps = [(kaTv[r0:r0 + 64, g, (cp + t) * 128:(cp + t + 1) * 128], kaT) for t in range(5)]
                band_unit(Sbp, Obp, u, 128, h, qaTv[r0:r0 + 64, g, cp * 128:(cp + 1) * 128], k_aps, [va[cp + t] for t in range(5)], ost[cp], 128)
                u += 1
        P.barrier()
        chunks = []
        for kc in range(4 * n + 4):
            mf = None
            if kc >= 4 * n:
                mf = (lambda kt, kc=kc, n=n: (Mv[:, (kc - 4 * n) * 4 + kt, :], M))
            chunks.append((S["KT"][:, :, kc * 512:(kc + 1) * 512].rearrange("h p t -> p h t"), S["VS"][kc * 512:(kc + 1) * 512, :], 512, mf))
        fox(512, lambda h: qbTv[:, h, :], chunks, ost)
        for t in range(4):
            r0 = n * 512 + t * 128
            P.dma("sync", lambda e, t=t, r0=r0: e.dma_start(out=S["OT"][r0:r0 + 128, :], in_=ost[t][:]), reads=[ost[t].b])
        P.barrier()

    pp = psum_proj()
    X = load_x(I["xs"], 0)
    HT = rms_tile(k, X, C["gmix"], pp["tr"])
    kaTn = get(k, "kaTn", 4 * 128, BF16)
    kaTnv = kaTn.v("p (g t) -> p g t", t=128)
    KAO, VAO = ka_va(pp, HT, None, (kaTnv, 0, kaTn), None)
    KAN = get(k, "KAN", 512)
    VAN = get(k, "VAN", 512)
    P.op(V_, lambda e: e.tensor_copy(out=KAN[:], in_=KAO[:]), reads=[KAO.b], writes=[KAN.b])
    P.op(V_, lambda e: e.tensor_copy(out=VAN[:], in_=VAO[:]), reads=[VAO.b], writes=[VAN.b])
    qaTs = get(k, "qaTs", 4 * 128, BF16)
    qbTs = get(k, "qbTs", NH * 128, BF16, parts=70)
    qaTsv = qaTs.v("p (g t) -> p g t", t=128)
    qbTsv = qbTs.v("p (h t) -> p h t", t=128)
    q_side(pp, HT, C["U2"], C["carry2"], False, (qaTsv[:, :, :], qaTs), (qbTsv[:, :, :], qbTs))
    for sbi in range(2):
        p0 = sbi * 64
        P.dma("sync", lambda e, sbi=sbi: e.dma_start(out=O["o_aks"][sbi, 0:448, :], in_=I["cak"][sbi, 64:512, :]))
        P.dma("sync", lambda e, sbi=sbi: e.dma_start(out=O["o_avs"][sbi, 0:448, :], in_=I["cav"][sbi, 64:512, :]))
        P.dma("sync", lambda e, sbi=sbi, p0=p0: e.dma_start(out=O["o_aks"][sbi, 448:512, :], in_=KAN[p0:p0 + 64, :]), reads=[KAN.b])
        P.dma("sync", lambda e, sbi=sbi, p0=p0: e.dma_start(out=O["o_avs"][sbi, 448:512, :], in_=VAN[p0:p0 + 64, :]), reads=[VAN.b])
        for t in range(4):
            KC_ = fo[0]
            P.dma("sync", lambda e, sbi=sbi, t=t: e.dma_start(out=KC_[:], in_=I["cak"][sbi, t * 128:(t + 1) * 128, :]), writes=[KC_.b])
            tr = to_T4(pp, KC_, None, 0)
            P.op(S_, lambda e, t=t, tr=tr: e.copy(out=kaTv[:, :, t * 128:(t + 1) * 128], in_=tr[:, 0:512].rearrange("p (g t) -> p g t", t=128)),
                 reads=[tr.b], writes=[kaT.b])
            VC_ = fo[1]
            P.dma("sync", lambda e, sbi=sbi, t=t: e.dma_start(out=VC_[:], in_=I["cav"][sbi, t * 128:(t + 1) * 128, :]), writes=[VC_.b])
            VA = va[t]
            vav = VA.v("p (h c) -> p h c", c=65)
            P.op(G_, lambda e, vav=vav: e.tensor_copy(out=vav[:, :, 0:HD], in_=VC_.v("p (h d) -> p h d", d=HD)), reads=[VC_.b], writes=[VA.b])
            ones_col(VA, vav)
        P.op(V_, lambda e, p0=p0: e.tensor_copy(out=kaTv[:, :, 512:576], in_=kaTnv[:, :, p0:p0 + 64]), reads=[kaTn.b], writes=[kaT.b])
        VA = va[4]
        vav = VA.v("p (h c) -> p h c", c=65)
        P.op(V_, lambda e, p0=p0, vav=vav: e.tensor_copy(out=vav[0:64, :, 0:HD], in_=VAN.v("p (h d) -> p h d", d=HD)[p0:p0 + 64, :, :]),
             reads=[VAN.b], writes=[VA.b])
        ones_col(VA, vav)
        P.barrier()
        Sbp, Obp = band(0, None, None, None, 0)
        for h in range(NH):
            g, r0 = h // 2, (h % 2) * 64
            k_aps = [(kaTv[r0:r0 + 64, g, t * 128:t * 128 + (64 if t == 4 else 128)], kaT) for t in range(5)]
            band_unit(Sbp, Obp, h, 64, h, qaTsv[r0:r0 + 64, g, p0:p0 + 64], k_aps, [va[t] for t in range(5)], ost[0], 64)
        P.barrier()
        chunks = []
        for kc in range(8):
            chunks.append((S["KTS"][sbi, :, :, kc * 512:(kc + 1) * 512].rearrange("h p t -> p h t"), S["VSS"][sbi, kc * 512:(kc + 1) * 512, :], 512, None))
        chunks.append((S["KTS"][sbi, :, :, 4096:4160].rearrange("h p t -> p h t"), S["VSS"][sbi, 4096:4160, :], 64,
                       lambda kt: (Uadd[0:64, 0:64], Uadd)))
        fox(64, lambda h, p0=p0: qbTsv[:, h, p0:p0 + 64], chunks, ost)
        P.dma("sync", lambda e, sbi=sbi: e.dma_start(out=S["OT"][2048 + sbi * 64:2048 + sbi * 64 + 64, :], in_=ost[0][0:64, :]), reads=[ost[0].b])
        P.barrier()
        pp = psum_proj()


def phase_merge(k):
    k.tag = ""
    P, C, I, O, S = k.P, k.C, k.I, k.O, k.S
    hxT = get(k, "hxT", 8 * NTOK, BF16)
    combT = get(k, "combT", NTOK, BF16, parts=32)
    hxTv = hxT.v("p (k t) -> p k t", t=NTOK)
    Wg = get(k, "Wg", 8 * 2048, BF16)
    Wgv = Wg.v("p (k c) -> p k c", c=2048)
    wpa = get(k, "wpa", 4 * D, BF16)
    wpb = get(k, "wpb", 4 * D, BF16)
    wo = get(k, "wo", 8 * D, BF16)
    Wr = get(k, "Wr", 8 * 36)
    Wrv = Wr.v("p (k c) -> p k c", c=36)
    wpav, wpbv, wov = wpa.v("p (k c) -> p k c", c=D), wpb.v("p (k c) -> p k c", c=D), wo.v("p (k c) -> p k c", c=D)
    for kc in range(8):
        P.dma(G_, lambda e, kc=kc: e.dma_start(out=Wgv[:, kc, :], in_=I["w_in"][kc * 128:(kc + 1) * 128, C_G:C_G + 2048]), writes=[Wg.b])
        P.dma(G_, lambda e, kc=kc: e.dma_start(out=wov[:, kc, :], in_=I["w_o"][kc * 128:(kc + 1) * 128, :]), writes=[wo.b])
        P.dma("sync", lambda e, kc=kc: e.dma_start(out=Wrv[:, kc, 0:4], in_=I["w_rg"][kc * 128:(kc + 1) * 128, :]), writes=[Wr.b])
        for g in range(4):
            P.dma("sync", lambda e, kc=kc, g=g: e.dma_start(out=Wrv[:, kc, 4 + 8 * g:12 + 8 * g], in_=I["w_re"][g, kc * 128:(kc + 1) * 128, :]), writes=[Wr.b])
    for kc in range(4):
        P.dma(G_, lambda e, kc=kc: e.dma_start(out=wpav[:, kc, :], in_=I["w_pa"][kc * 128:(kc + 1) * 128, :]), writes=[wpa.b])
        P.dma(G_, lambda e, kc=kc: e.dma_start(out=wpbv[:, kc, :], in_=I["w_pb"][kc * 128:(kc + 1) * 128, :]), writes=[wpb.b])
    tr = pbank(k, "tr", 0, 1, BF16)
    gp = pbank(k, "gp", 1, 4)
    pa = pbank(k, "pa", 5, 2)
    sm = pbank(k, "sm", 7)
    xin = [get(k, "xin%d" % i, D) for i in range(2)]
    gT = get(k, "gT", 2048, BF16)
    OTt = get(k, "OTt", 1024, BF16)
    oT = get(k, "oT", 1024, BF16)
    ta = get(k, "ta", D)
    tb = get(k, "tb", D)
    uT = get(k, "uT", D, BF16)
    y1 = get(k, "y1", D)
    hxf = get(k, "hxf", D)
    hxTf = get(k, "hxTf", D)
    junk = get(k, "junk", D, BF16)
    ss, rs = get(k, "ss", 1), get(k, "rs", 1)
    sm_names = ["lg", "gmax", "ngmax", "ohg", "eg", "sume", "pg", "tmp32", "fsel", "m1", "oh1", "fm", "m2", "oh2", "dd", "e2", "den", "p1", "p2", "t8", "comb8", "comb"]
    sm_cols = [36, 1, 1, 4, 4, 1, 1, 32, 8, 1, 8, 8, 1, 8, 1, 1, 1, 1, 1, 8, 8, 32]
    R = {n: get(k, "r_" + n, c) for n, c in zip(sm_names, sm_cols)}
    ident, identf = C["ident"], C["identf"]
    for ti in range(17):
        src, r0 = (I["xown"], ti * 128) if ti < 16 else (I["xs"], 0)
        tok0 = ti * 128
        X = xin[ti % 2]
        P.dma("sync", lambda e, X=X, src=src, r0=r0: e.dma_start(out=X[:], in_=src[r0:r0 + 128, :]), writes=[X.b])
        P.dma("sync", lambda e, tok0=tok0: e.dma_start(out=OTt[:], in_=S["OT"][tok0:tok0 + 128, :]), writes=[OTt.b])
        HT = rms_tile(k, X, C["gmix"], tr)
        for gc in range(16):
            for kc in range(8):
                P.op(PE_, lambda e, gc=gc, kc=kc: e.matmul(gp[:, gc * 128:(gc + 1) * 128], lhsT=Wgv[:, kc, gc * 128:(gc + 1) * 128],
                                                           rhs=HT[:, kc * 128:(kc + 1) * 128], start=(kc == 0), stop=(kc == 7)),
                     reads=[Wg.b, HT.b], writes=[gp.b])
        P.op(S_, lambda e: e.activation(out=gT[:], in_=gp[:, 0:2048], func=AF.Sigmoid), reads=[gp.b], writes=[gT.b])
        for kk in range(8):
            P.op(PE_, lambda e, kk=kk: e.transpose(tr[:, kk * 128:(kk + 1) * 128], OTt[:, kk * 128:(kk + 1) * 128], ident[:]),
                 reads=[OTt.b, ident.b], writes=[tr.b])
        P.op(S_, lambda e: e.copy(out=oT[:], in_=tr[:, 0:1024]), reads=[tr.b], writes=[oT.b])
        for dc in range(8):
            for kk in range(4):
                P.op(PE_, lambda e, dc=dc, kk=kk: e.matmul(pa[:, dc * 128:(dc + 1) * 128], lhsT=wpav[:, kk, dc * 128:(dc + 1) * 128],
                                                           rhs=oT[:, kk * 128:(kk + 1) * 128], start=(kk == 0), stop=(kk == 3)),
                     reads=[wpa.b, oT.b], writes=[pa.b])
        for dc in range(8):
            for kk in range(4):
                P.op(PE_, lambda e, dc=dc, kk=kk: e.matmul(gp[:, 1024 + dc * 128:1024 + (dc + 1) * 128], lhsT=wpbv[:, kk, dc * 128:(dc + 1) * 128],
                                                           rhs=oT[:, (4 + kk) * 128:(5 + kk) * 128], start=(kk == 0), stop=(kk == 3)),
                     reads=[wpb.b, oT.b], writes=[gp.b])
        P.op(V_, lambda e: e.tensor_tensor(out=ta[:], in0=pa[:, 0:1024], in1=gT[:, 0:1024], op=ALU.mult), reads=[pa.b, gT.b], writes=[ta.b])
        P.op(V_, lambda e: e.tensor_tensor(out=tb[:], in0=gp[:, 1024:2048], in1=gT[:, 1024:2048], op=ALU.mult), reads=[gp.b, gT.b], writes=[tb.b])
        P.op(G_, lambda e: e.tensor_tensor(out=uT[:], in0=ta[:], in1=tb[:], op=ALU.add), reads=[ta.b, tb.b], writes=[uT.b])
        for half in range(2):
            for kk in range(8):
                P.op(PE_, lambda e, half=half, kk=kk: e.matmul(gp[:, half * 512:(half + 1) * 512], lhsT=uT[:, kk * 128:(kk + 1) * 128],
                                                               rhs=wov[:, kk, half * 512:(half + 1) * 512], start=(kk == 0), stop=(kk == 7)),
                     reads=[uT.b, wo.b], writes=[gp.b])
        P.op(V_, lambda e, X=X: e.tensor_tensor(out=y1[:], in0=gp[:, 0:1024], in1=X[:], op=ALU.add), reads=[gp.b, X.b], writes=[y1.b])
        P.dma("sync", lambda e, tok0=tok0: e.dma_start(out=S["Y1"][tok0:tok0 + 128, :], in_=y1[:]), reads=[y1.b])
        if os.environ.get("KNOROUTER"):
            P.dma(G_, lambda e, tok0=tok0: e.dma_start(out=O["o_dbg"][tok0:tok0 + 128, :], in_=OTt[:]), reads=[OTt.b])
            dsty = O["o_y"][tok0:tok0 + 128, :] if ti < 16 else O["o_ys"][:, :]
            P.dma("sync", lambda e, dsty=dsty: e.dma_start(out=dsty, in_=y1[:]), reads=[y1.b])
            continue
        P.op(S_, lambda e: e.activation(out=junk[:], in_=y1[:], func=AF.Square, accum_out=ss[:, 0:1]), reads=[y1.b], writes=[junk.b, ss.b])
        rsqrt_act(k, rs, ss, D)
        P.op(V_, lambda e: e.scalar_tensor_tensor(out=hxf[:], in0=y1[:], scalar=rs[:, 0:1], in1=C["gffn"][:], op0=ALU.mult, op1=ALU.mult),
             reads=[y1.b, rs.b, C["gffn"].b], writes=[hxf.b])
        for kc in range(8):
            P.op(PE_, lambda e, kc=kc: e.matmul(pa[:, kc * 128:(kc + 1) * 128], lhsT=hxf[:, kc * 128:(kc + 1) * 128], rhs=identf[:], start=True, stop=True),
                 reads=[hxf.b, identf.b], writes=[pa.b])
        P.op(S_, lambda e: e.copy(out=hxTf[:], in_=pa[:, 0:1024]), reads=[pa.b], writes=[hxTf.b])
        P.op(V_, lambda e, tok0=tok0: e.tensor_copy(out=hxTv[:, :, tok0:tok0 + 128], in_=hxTf.v("p (k t) -> p k t", t=128)),
             reads=[hxTf.b], writes=[hxT.b])
        for kc in range(8):
            P.op(PE_, lambda e, kc=kc: e.matmul(sm[:, 0:36], lhsT=hxTf[:, kc * 128:(kc + 1) * 128], rhs=Wrv[:, kc, :], start=(kc == 0), stop=(kc == 7)),
                 reads=[hxTf.b, Wr.b], writes=[sm.b])
        r = R
        def vo(fn, reads, writes):
            P.op(V_, fn, reads=[x.b for x in reads], writes=[x.b for x in writes])
        vo(lambda e: e.tensor_tensor(out=r["lg"][:, 0:4], in0=sm[:, 0:4], in1=C["brg"][:], op=ALU.add), [sm, C["brg"]], [r["lg"]])
        vo(lambda e: e.tensor_tensor(out=r["lg"][:, 4:36], in0=sm[:, 4:36], in1=C["bre"][:], op=ALU.add), [sm, C["bre"], r["lg"]], [r["lg"]])
        vo(lambda e: e.tensor_reduce(out=r["gmax"][:], in_=r["lg"][:, 0:4], axis=AX.X, op=ALU.max), [r["lg"]], [r["gmax"]])
        vo(lambda e: e.tensor_scalar(out=r["ohg"][:], in0=r["lg"][:, 0:4], scalar1=r["gmax"][:, 0:1], scalar2=None, op0=ALU.is_ge), [r["lg"], r["gmax"]], [r["ohg"]])
        vo(lambda e: e.tensor_scalar(out=r["ngmax"][:], in0=r["gmax"][:], scalar1=-1.0, scalar2=None, op0=ALU.mult), [r["gmax"]], [r["ngmax"]])
        P.op(S_, lambda e: e.activation(out=r["eg"][:], in_=r["lg"][:, 0:4], func=AF.Exp, bias=r["ngmax"][:, 0:1], scale=1.0, accum_out=r["sume"][:, 0:1]),
             reads=[r["lg"].b, r["ngmax"].b], writes=[r["eg"].b, r["sume"].b])
        vo(lambda e: e.reciprocal(out=r["pg"][:], in_=r["sume"][:]), [r["sume"]], [r["pg"]])
        vo(lambda e: e.tensor_tensor(out=r["tmp32"].v("p (g e) -> p g e", e=8), in0=r["lg"][:, 4:36].rearrange("p (g e) -> p g e", e=8),
                                     in1=r["ohg"][:].unsqueeze(2).to_broadcast([128, 4, 8]), op=ALU.mult), [r["lg"], r["ohg"]], [r["tmp32"]])
        vo(lambda e: e.tensor_reduce(out=r["fsel"][:], in_=r["tmp32"].v("p (g e) -> p e g", e=8), axis=AX.X, op=ALU.add), [r["tmp32"]], [r["fsel"]])
        vo(lambda e: e.tensor_reduce(out=r["m1"][:], in_=r["fsel"][:], axis=AX.X, op=ALU.max), [r["fsel"]], [r["m1"]])
        vo(lambda e: e.tensor_scalar(out=r["oh1"][:], in0=r["fsel"][:], scalar1=r["m1"][:, 0:1], scalar2=None, op0=ALU.is_ge), [r["fsel"], r["m1"]], [r["oh1"]])
        vo(lambda e: e.scalar_tensor_tensor(out=r["fm"][:], in0=r["oh1"][:], scalar=-1e30, in1=r["fsel"][:], op0=ALU.mult, op1=ALU.add), [r["oh1"], r["fsel"]], [r["fm"]])
        vo(lambda e: e.tensor_reduce(out=r["m2"][:], in_=r["fm"][:], axis=AX.X, op=ALU.max), [r["fm"]], [r["m2"]])
        vo(lambda e: e.tensor_scalar(out=r["oh2"][:], in0=r["fm"][:], scalar1=r["m2"][:, 0:1], scalar2=None, op0=ALU.is_ge), [r["fm"], r["m2"]], [r["oh2"]])
        vo(lambda e: e.tensor_sub(out=r["dd"][:], in0=r["m2"][:], in1=r["m1"][:]), [r["m2"], r["m1"]], [r["dd"]])
        P.op(S_, lambda e: e.activation(out=r["e2"][:], in_=r["dd"][:], func=AF.Exp), reads=[r["dd"].b], writes=[r["e2"].b])
        vo(lambda e: e.tensor_scalar(out=r["den"][:], in0=r["e2"][:], scalar1=1.0, scalar2=None, op0=ALU.add), [r["e2"]], [r["den"]])
        vo(lambda e: e.reciprocal(out=r["p1"][:], in_=r["den"][:]), [r["den"]], [r["p1"]])
        vo(lambda e: e.tensor_tensor(out=r["p2"][:], in0=r["e2"][:], in1=r["p1"][:], op=ALU.mult), [r["e2"], r["p1"]], [r["p2"]])
        vo(lambda e: e.tensor_scalar(out=r["t8"][:], in0=r["oh1"][:], scalar1=r["p1"][:, 0:1], scalar2=None, op0=ALU.mult), [r["oh1"], r["p1"]], [r["t8"]])
        vo(lambda e: e.scalar_tensor_tensor(out=r["comb8"][:], in0=r["oh2"][:], scalar=r["p2"][:, 0:1], in1=r["t8"][:], op0=ALU.mult, op1=ALU.add),
           [r["oh2"], r["p2"], r["t8"]], [r["comb8"]])
        vo(lambda e: e.tensor_scalar(out=r["comb8"][:], in0=r["comb8"][:], scalar1=r["pg"][:, 0:1], scalar2=None, op0=ALU.mult), [r["comb8"], r["pg"]], [r["comb8"]])
        vo(lambda e: e.tensor_tensor(out=r["comb"].v("p (g e) -> p g e", e=8), in0=r["ohg"][:].unsqueeze(2).to_broadcast([128, 4, 8]),
                                     in1=r["comb8"][:].unsqueeze(1).to_broadcast([128, 4, 8]), op=ALU.mult), [r["ohg"], r["comb8"]], [r["comb"]])
        P.op(PE_, lambda e: e.matmul(sm[0:32, 64:192], lhsT=r["comb"][:], rhs=identf[:], start=True, stop=True), reads=[r["comb"].b, identf.b], writes=[sm.b])
        P.op(V_, lambda e, tok0=tok0: e.tensor_copy(out=combT[:, tok0:tok0 + 128], in_=sm[0:32, 64:192]), reads=[sm.b], writes=[combT.b])


def phase_moe(k):
    k.tag = ""
    P, C, I, O, S = k.P, k.C, k.I, k.O, k.S
    hxT = get(k, "hxT", 8 * NTOK, BF16)
    combT = get(k, "combT", NTOK, BF16, parts=32)
    hxTv = hxT.v("p (k t) -> p k t", t=NTOK)
    yacc = get(k, "yacc", 17 * D)
    yv = yacc.v("p (a d) -> p a d", d=D)
    for ti in range(17):
        P.dma("sync", lambda e, ti=ti: e.dma_start(out=yv[:, ti, :], in_=S["Y1"][ti * 128:(ti + 1) * 128, :]), writes=[yacc.b])
    Sel = get(k, "Sel", 32 * 128, BF16, parts=32)
    Selv = Sel.v("p (e c) -> p e c", c=128)
    P.op(G_, lambda e: e.memset(Sel[:], 0.0), writes=[Sel.b])
    P.op(G_, lambda e: e.affine_select(out=Selv, in_=Selv, pattern=[[-1, 32], [0, 128]], compare_op=ALU.not_equal, fill=1.0,
                                       base=0, channel_multiplier=1), reads=[Sel.b], writes=[Sel.b])
    W13 = [get(k, "W13_%d" % i, 2 * 8 * 256, BF16) for i in range(2)]
    W2 = [get(k, "W2_%d" % i, 2 * D, BF16) for i in range(2)]
    s1 = [get(k, "s1_%d" % i, 256) for i in range(2)]
    tt_ = [get(k, "tt_%d" % i, 256) for i in range(2)]
    hid = [get(k, "hid%d" % i, 256, BF16) for i in range(2)]
    h13 = [pbank(k, "h13_%d" % i, i) for i in range(2)]
    cbb = pbank(k, "cbb", 2)
    yp = [pbank(k, "yp%d" % i, 3 + 2 * i, 2) for i in range(2)]

    def load_w(gi):
        b = gi % 2
        Wv = W13[b].v("p (e k c) -> p e k c", k=8, c=256)
        W2v = W2[b].v("p (e d) -> p e d", d=D)
        for el in range(2):
            e_ = 2 * gi + el
            P.dma(G_, lambda e, el=el, e_=e_, Wv=Wv: e.dma_start(out=Wv[:, el, :, 0:128], in_=I["w1"][e_].rearrange("(k p) f -> p k f", p=128)), writes=[W13[b].b])
            P.dma(G_, lambda e, el=el, e_=e_, Wv=Wv: e.dma_start(out=Wv[:, el, :, 128:256], in_=I["w3"][e_].rearrange("(k p) f -> p k f", p=128)), writes=[W13[b].b])
            P.dma(G_, lambda e, el=el, e_=e_, W2v=W2v: e.dma_start(out=W2v[:, el, :], in_=I["w2"][e_]), writes=[W2[b].b])

    blocks = [(i * 256, 256) for i in range(8)] + [(2048, 128)]
    load_w(0)

    def down(item):
        (tok0, nt, el, HID, W2v, b) = item
        for tt in range(nt // 128):
            for half in range(2):
                P.op(PE_, lambda e, tt=tt, half=half: e.matmul(yp[tt][:, half * 512:(half + 1) * 512], lhsT=HID[:, tt * 128:(tt + 1) * 128],
                                                               rhs=W2v[:, el, half * 512:(half + 1) * 512], start=(el == 0), stop=(el == 1)),
                     reads=[HID.b, W2[b].b], writes=[yp[tt].b])
        if el == 1:
            for tt in range(nt // 128):
                ti = tok0 // 128 + tt
                P.op(V_, lambda e, tt=tt, ti=ti: e.tensor_tensor(out=yv[:, ti, :], in0=yp[tt][:, 0:1024], in1=yv[:, ti, :], op=ALU.add),
                     reads=[yp[tt].b, yacc.b], writes=[yacc.b])

    def up(u, tok0, nt, el, e_, Wv, b):
        H, S1, TT, HID = h13[u % 2], s1[u % 2], tt_[u % 2], hid[u % 2]
        cb = cbb[:, (u % 2) * 256:(u % 2) * 256 + nt]
        for (c0, o0) in ((0, 0), (128, 256)):
            for kc in range(8):
                P.op(PE_, lambda e, c0=c0, o0=o0, kc=kc: e.matmul(H[:, o0:o0 + nt], lhsT=Wv[:, el, kc, c0:c0 + 128], rhs=hxTv[:, kc, tok0:tok0 + nt],
                                                                  start=(kc == 0), stop=(kc == 7)), reads=[W13[b].b, hxT.b], writes=[H.b])
        P.op(PE_, lambda e: e.matmul(cb, lhsT=Selv[:, e_, :], rhs=combT[:, tok0:tok0 + nt], start=True, stop=True),
             reads=[Sel.b, combT.b], writes=[cbb.b])
        P.op(S_, lambda e: e.activation(out=S1[:, 0:nt], in_=H[:, 0:nt], func=AF.Silu), reads=[H.b], writes=[S1.b])
        P.op(V_, lambda e: e.tensor_tensor(out=TT[:, 0:nt], in0=H[:, 256:256 + nt], in1=S1[:, 0:nt], op=ALU.mult),
             reads=[H.b, S1.b], writes=[TT.b])
        P.op(V_, lambda e: e.tensor_tensor(out=HID[:, 0:nt], in0=cb, in1=TT[:, 0:nt], op=ALU.mult),
             reads=[cbb.b, TT.b], writes=[HID.b])
        return HID

    u = 0
    for gi in range(16):
        if gi + 1 < 16:
            load_w(gi + 1)
        b = gi % 2
        Wv = W13[b].v("p (e k c) -> p e k c", k=8, c=256)
        W2v = W2[b].v("p (e d) -> p e d", d=D)
        pend = []
        for (tok0, nt) in blocks:
            for el in range(2):
                HID = up(u, tok0, nt, el, 2 * gi + el, Wv, b)
                u += 1
                if pend:
                    down(pend.pop(0))
                pend.append((tok0, nt, el, HID, W2v, b))
        while pend:
            down(pend.pop(0))
    for ti in range(17):
        dst = O["o_y"][ti * 128:(ti + 1) * 128, :] if ti < 16 else O["o_ys"][:, :]
        P.dma("sync", lambda e, ti=ti, dst=dst: e.dma_start(out=dst, in_=yv[:, ti, :]), reads=[yacc.b])


_CACHE = {}


def kernel(**inp):
    f = lambda a: np.ascontiguousarray(np.asarray(a, dtype=np.float32))
    x_prompt = f(inp["x_prompt"])
    x_sample = f(inp["x_sample"])
    if "nc" not in _CACHE:
        _CACHE["nc"] = build_program()
    nc = _CACHE["nc"]
    cak, cav = f(inp["cache_a_k"])[0].reshape(16, 512, 512), f(inp["cache_a_v"])[0].reshape(16, 512, 512)
    cbk, cbv = f(inp["cache_b_k"])[0].reshape(16, 4096, 512), f(inp["cache_b_v"])[0].reshape(16, 4096, 512)
    cblf = f(inp["cache_b_logf"])[0]
    shared = {
        "g_mix": f(inp["g_mix"]), "w_in": f(inp["w_in"])[0], "b_f": f(inp["b_f"]),
        "q_norm_a": f(inp["q_norm_a"]), "k_norm_a": f(inp["k_norm_a"]), "q_norm_b": f(inp["q_norm_b"]), "k_norm_b": f(inp["k_norm_b"]),
        "rel_bias": f(inp["rel_bias"])[0], "w_pa": f(inp["w_pa"])[0], "w_pb": f(inp["w_pb"])[0], "w_o": f(inp["w_o"])[0],
        "g_ffn": f(inp["g_ffn"]), "w_rg": f(inp["w_rg"])[0], "b_rg": f(inp["b_rg"]), "w_re": f(inp["w_re"])[0],
        "b_re": f(inp["b_re"]).reshape(1, 32), "w1": f(inp["w1"])[0], "w3": f(inp["w3"])[0], "w2": f(inp["w2"])[0],
    }
    in_maps = []
    for c in range(8):
        b, j = c // 4, c % 4
        blocks = [4 * n + j for n in range(4)]
        xown = np.concatenate([x_prompt[b, g * 512:(g + 1) * 512] for g in blocks], axis=0)
        xhalo = np.concatenate([x_prompt[b, (g - 1) * 512:g * 512] if g > 0 else np.zeros((512, D), np.float32) for g in blocks], axis=0)
        wm = np.zeros((64, 4), np.float32)
        hv = np.ones((1, 4), np.float32)
        for n in range(4):
            wm[:4 * blocks[n], n] = 1.0
            if blocks[n] == 0:
                hv[0, n] = 0.0
        m = dict(shared)
        m.update({
            "xfull": x_prompt[b], "xown": xown, "xhalo": xhalo, "xs": x_sample[2 * c:2 * c + 2].reshape(128, D),
            "cak": cak[2 * c:2 * c + 2], "cav": cav[2 * c:2 * c + 2], "cbk": cbk[2 * c:2 * c + 2], "cbv": cbv[2 * c:2 * c + 2],
            "cblf": cblf[2 * c:2 * c + 2],
            "wmeta": wm.reshape(1, 256), "hvalid": hv, "qpos": (j * 512 + np.arange(512, dtype=np.float32)).reshape(1, 512),
        })
        in_maps.append(m)
    res = run_bass_kernel_spmd(nc, in_maps, core_ids=list(range(8)))
    R = res.results
    _CACHE["last"] = R
    y_p = np.zeros((2, SEQ, D), np.float32)
    for c in range(8):
        b, j = c // 4, c % 4
        for n in range(4):
            g = 4 * n + j
            y_p[b, g * 512:(g + 1) * 512] = R[c]["o_y"][n * 512:(n + 1) * 512]
    y_s = np.concatenate([R[c]["o_ys"].reshape(2, 64, D) for c in range(8)], axis=0)
    a_k_p = np.stack([R[3]["o_ak"], R[7]["o_ak"]]).reshape(1, 2, 512, NH, HD)
    a_v_p = np.stack([R[3]["o_av"], R[7]["o_av"]]).reshape(1, 2, 512, NH, HD)
    b_k_p = np.stack([R[0]["o_bk"], R[4]["o_bk"]]).reshape(1, 2, SEQ, NH, HD)
    b_v_p = np.stack([R[0]["o_bv"], R[4]["o_bv"]]).reshape(1, 2, SEQ, NH, HD)
    b_lf_p = np.stack([R[0]["o_blf"], R[4]["o_blf"]]).reshape(1, 2, SEQ, NH)
    a_k_s = np.concatenate([R[c]["o_aks"] for c in range(8)], axis=0).reshape(1, 16, 512, NH, HD)
    a_v_s = np.concatenate([R[c]["o_avs"] for c in range(8)], axis=0).reshape(1, 16, 512, NH, HD)
    b_k_s = np.concatenate([R[c]["o_bks"].reshape(2, 64, 512) for c in range(8)], axis=0).reshape(1, 16, 64, NH, HD)
    b_v_s = np.concatenate([R[c]["o_bvs"].reshape(2, 64, 512) for c in range(8)], axis=0).reshape(1, 16, 64, NH, HD)
    b_lf_s = np.concatenate([R[c]["o_blfs"].reshape(2, 64, NH) for c in range(8)], axis=0).reshape(1, 16, 64, NH)
    return (y_p, y_s, a_k_p, a_v_p, b_k_p, b_v_p, b_lf_p, a_k_s, a_v_s, b_k_s, b_v_s, b_lf_s)
```

```python
import contextlib
import os
import numpy as np
import concourse.bass as bass
import concourse.mybir as mybir
from concourse.bass_utils import run_bass_kernel_spmd

F32 = mybir.dt.float32
BF16 = mybir.dt.bfloat16
AF = mybir.ActivationFunctionType
ALU = mybir.AluOpType
AX = mybir.AxisListType

D = 1024
SEQ = 8192
NH = 8
HD = 64
EPS = 1e-6
IN_COLS = 5128
C_QA, C_KA, C_VA, C_QB, C_KB, C_VB, C_F, C_G = 0, 512, 1024, 1536, 2048, 2560, 3072, 3080

ENGS = ["tensor", "scalar", "vector", "gpsimd", "sync"]
EPOCH = 4000


class Buf:
    __slots__ = ("name", "writers", "readers")

    def __init__(self, name=""):
        self.name = name
        self.writers = []
        self.readers = []


class Prog:
    def __init__(self, nc, same_engine_wait=True, n_dma_sems=8):
        self.nc = nc
        self.same = same_engine_wait
        self.streams = {e: [] for e in ENGS}
        self.count = {e: 0 for e in ENGS}
        self.epoch = {e: 0 for e in ENGS}
        self.sems = {}
        self.known = {e: {} for e in ENGS}
        self.n_dma_sems = n_dma_sems
        self.dma_rr = {e: 0 for e in ENGS}
        self.dma_cnt = {}
        self.dma_ep = {}
        self._ctx = []

    def _sem(self, key):
        if key not in self.sems:
            cm = self.nc.semaphore("s_" + "_".join(str(k) for k in key))
            h = cm.__enter__()
            self._ctx.append(cm)
            self.sems[key] = h
        return self.sems[key]

    def _waits(self, eng, deps):
        need = {}
        for (key, v) in deps:
            if key[0] == eng and len(key) == 2 and (eng == "tensor" or not self.same):
                continue
            if self.known[eng].get(key, 0) >= v:
                continue
            if need.get(key, 0) < v:
                need[key] = v
        out = []
        for key, v in need.items():
            self.known[eng][key] = v
            out.append((self._sem(key), v))
        return out

    @staticmethod
    def _deps(reads, writes):
        deps = []
        for b in reads:
            deps += b.writers
        for b in writes:
            deps += b.writers
            deps += b.readers
        return deps

    @staticmethod
    def _commit(ev, reads, writes):
        for b in writes:
            b.writers = [ev]
            b.readers = []
        for b in reads:
            if b not in writes:
                b.readers = [r for r in b.readers if r[0] != ev[0]] + [ev]

    def op(self, eng, fn, reads=(), writes=()):
        deps = self._deps(reads, writes)
        waits = self._waits(eng, deps)
        if self.count[eng] >= EPOCH:
            self.epoch[eng] += 1
            self.count[eng] = 0
        key = (eng, self.epoch[eng])
        sem = self._sem(key)
        self.count[eng] += 1
        ev = (key, self.count[eng])
        self.streams[eng].append((waits, fn, sem, 1))
        self._commit(ev, reads, writes)
        return ev

    def dma(self, eng, fn, reads=(), writes=()):
        deps = self._deps(reads, writes)
        i = self.dma_rr[eng]
        self.dma_rr[eng] = (i + 1) % self.n_dma_sems
        ep = self.dma_ep.get((eng, i), 0)
        key = (eng, "d", i, ep)
        prev = self.dma_cnt.get(key, 0)
        if prev:
            deps = list(deps) + [(key, prev)]
        if prev >= EPOCH:
            ep += 1
            self.dma_ep[(eng, i)] = ep
            key = (eng, "d", i, ep)
            prev = 0
        waits = self._waits(eng, deps)
        sem = self._sem(key)
        self.dma_cnt[key] = prev + 16
        ev = (key, prev + 16)
        self.streams[eng].append((waits, fn, sem, 16))
        self._commit(ev, reads, writes)
        return ev

    def barrier(self):
        evs = []
        for e in ENGS:
            if self.count[e] > 0:
                evs.append(((e, self.epoch[e]), self.count[e]))
        for key, v in self.dma_cnt.items():
            evs.append((key, v))
        for e in ENGS:
            waits = self._waits(e, [ev for ev in evs if not (ev[0][0] == e and len(ev[0]) == 2)])
            if waits:
                self.streams[e].append((waits, None, None, 0))

    def finish(self):
        nc = self.nc
        final = [(self._sem(key), v) for key, v in self.dma_cnt.items()]
        streams = self.streams

        def run(e, name):
            for (waits, fn, sem, inc) in streams[name]:
                for (s, v) in waits:
                    e.wait_ge(s, v)
                if fn is not None:
                    fn(e).then_inc(sem, inc)
            if name == "sync":
                for (s, v) in final:
                    e.wait_ge(s, v)

        with nc.Block() as block:
            @block.sync
            def _(e):
                run(e, "sync")

            @block.tensor
            def _(e):
                run(e, "tensor")

            @block.scalar
            def _(e):
                run(e, "scalar")

            @block.vector
            def _(e):
                run(e, "vector")

            @block.gpsimd
            def _(e):
                run(e, "gpsimd")

    def close(self):
        for cm in reversed(self._ctx):
            cm.__exit__(None, None, None)
        self._ctx = []


class T:
    def __init__(self, ap, name):
        self.ap = ap
        self.b = Buf(name)

    def __getitem__(self, k):
        return self.ap[k]

    def v(self, pattern, **kw):
        return self.ap.rearrange(pattern, **kw)


class Arena:
    def __init__(self, ap2d):
        self.ap = ap2d
        self.n = ap2d.shape[1]
        self.top = 0

    def alloc(self, name, cols, dt=F32, parts=128):
        words = cols if dt == F32 else (cols + 1) // 2
        a = self.top
        self.top += words
        assert self.top <= self.n, ("SBUF arena overflow", name, self.top, self.n)
        v = self.ap[:, a:a + words]
        if dt != F32:
            v = v.bitcast(dt)[:, 0:cols]
        if parts < 128:
            v = v[0:parts]
        return T(v, name)


NTOK = 2176
SARENA = 50500


class K:
    pass


def build_program():
    nc = bass.Bass("TRN2", target_bir_lowering=False)
    k = K()
    k.nc = nc

    def din(name, shape):
        return nc.dram_tensor(name, list(shape), F32, kind="ExternalInput").ap()

    def dout(name, shape):
        return nc.dram_tensor(name, list(shape), F32, kind="ExternalOutput").ap()

    I = k.I = {}
    for name, shape in [
        ("xfull", [SEQ, D]), ("xown", [2048, D]), ("xhalo", [2048, D]), ("xs", [128, D]),
        ("cak", [2, 512, 512]), ("cav", [2, 512, 512]), ("cbk", [2, 4096, 512]), ("cbv", [2, 4096, 512]),
        ("cblf", [2, 4096, NH]),
        ("g_mix", [1, D]), ("w_in", [D, IN_COLS]), ("b_f", [1, NH]),
        ("q_norm_a", [1, HD]), ("k_norm_a", [1, HD]), ("q_norm_b", [1, HD]), ("k_norm_b", [1, HD]),
        ("rel_bias", [NH, 513]), ("w_pa", [512, D]), ("w_pb", [512, D]), ("w_o", [D, D]),
        ("g_ffn", [1, D]), ("w_rg", [D, 4]), ("b_rg", [1, 4]), ("w_re", [4, D, 8]), ("b_re", [1, 32]),
        ("w1", [32, D, 128]), ("w3", [32, D, 128]), ("w2", [32, 128, D]),
        ("wmeta", [1, 256]), ("hvalid", [1, 4]), ("qpos", [1, 512]),
    ]:
        I[name] = din(name, shape)
    O = k.O = {}
    for name, shape in [
        ("o_y", [2048, D]), ("o_ys", [128, D]), ("o_ak", [512, 512]), ("o_av", [512, 512]),
        ("o_bk", [SEQ, 512]), ("o_bv", [SEQ, 512]), ("o_blf", [SEQ, NH]),
        ("o_aks", [2, 512, 512]), ("o_avs", [2, 512, 512]),
        ("o_bks", [128, 512]), ("o_bvs", [128, 512]), ("o_blfs", [128, NH]),
    ] + ([("o_dbg", [NTOK, 1024]), ("o_kt", [NH, 70, 256]), ("o_vs", [256, 520]), ("o_qb", [70, NH * 512])] if os.environ.get("KNOROUTER") else []):
        O[name] = dout(name, shape)
    S = k.S = {}
    S["KT"] = nc.dram_tensor("KTs", [NH, 70, SEQ], BF16).ap()
    S["VS"] = nc.dram_tensor("VSs", [SEQ, NH * 65], BF16).ap()
    S["KTS"] = nc.dram_tensor("KTSs", [2, NH, 70, 4224], BF16).ap()
    S["VSS"] = nc.dram_tensor("VSSs", [2, 4224, NH * 65], BF16).ap()
    S["OT"] = nc.dram_tensor("OTs", [NTOK, 1024], BF16).ap()
    S["Y1"] = nc.dram_tensor("Y1s", [NTOK, D], F32).ap()
    S["EXT"] = nc.dram_tensor("EXTs", [NH, 896], F32).ap()

    P = k.P = Prog(nc)
    with contextlib.ExitStack() as es:
        arena_t = es.enter_context(nc.sbuf_tensor("arena", [128, SARENA], F32))
        psum_t = es.enter_context(nc.psum_tensor("parena", [128, 4096], F32))
        k.A = Arena(arena_t[:, :])
        k.psum = psum_t
        setup_consts(k)
        base = k.A.top
        sel = os.environ.get("KPH", "full,own,merge,moe").split(",")
        for ph in (phase_full, phase_own, phase_merge, phase_moe):
            if ph.__name__[6:] not in sel:
                continue
            k.A.top = base
            k.pbuf = {}
            ph(k)
            P.barrier()
        P.finish()
        P.close()
    return nc


def pbank(k, name, b0, nb=1, dt=F32):
    v = k.psum[:, b0 * 512:(b0 + nb) * 512]
    if dt != F32:
        v = v.bitcast(dt)
    return T(v, name)


def bcast_rows(ap_row, n, off=0):
    return bass.AP(ap_row.tensor, ap_row.offset + off, [[0, 128], [1, n]])


V_, S_, G_, PE_ = "vector", "scalar", "gpsimd", "tensor"


def setup_consts(k):
    P, A, I = k.P, k.A, k.I
    C = k.C = {}
    for name, cols, dt in [("identf", 128, F32), ("ident", 128, BF16), ("U", 128, F32), ("U2", 128, F32), ("Ubf", 128, BF16),
                           ("ones", 128, F32), ("gmix", D, F32), ("gffn", D, F32), ("gqa", 512, F32), ("gka", 512, F32),
                           ("gqb", 512, F32), ("gkb", 512, F32), ("bf", NH, F32), ("wm", 256, F32), ("epsb", 2, F32),
                           ("hv", 4, F32), ("offs", 32, F32), ("carry2", NH, F32), ("brg", 4, F32), ("bre", 32, F32)]:
        C[name] = A.alloc(name, cols, dt)
    identf, ident, U, U2, Ubf, ones = C["identf"], C["ident"], C["U"], C["U2"], C["Ubf"], C["ones"]
    P.op(G_, lambda e: e.memset(identf[:], 0.0), writes=[identf.b])
    P.op(G_, lambda e: e.affine_select(out=identf[:], in_=identf[:], pattern=[[-1, 128]], compare_op=ALU.not_equal,
                                       fill=1.0, base=0, channel_multiplier=1), reads=[identf.b], writes=[identf.b])
    P.op(V_, lambda e: e.tensor_copy(out=ident[:], in_=identf[:]), reads=[identf.b], writes=[ident.b])
    P.op(G_, lambda e: e.memset(ones[:], 1.0), writes=[ones.b])
    P.op(G_, lambda e: e.affine_select(out=U[:], in_=ones[:], pattern=[[1, 128]], compare_op=ALU.is_ge, fill=0.0,
                                       base=0, channel_multiplier=-1), reads=[ones.b], writes=[U.b])
    P.op(V_, lambda e: e.tensor_copy(out=Ubf[:], in_=U[:]), reads=[U.b], writes=[Ubf.b])
    P.op(V_, lambda e: e.tensor_copy(out=U2[:], in_=U[:]), reads=[U.b], writes=[U2.b])
    P.op(V_, lambda e: e.memset(U2[0:64, 64:128], 0.0), reads=[U2.b], writes=[U2.b])
    P.op(G_, lambda e: e.memset(C["epsb"][:, 0:1], float(D * EPS)), writes=[C["epsb"].b])
    P.op(G_, lambda e: e.memset(C["epsb"][:, 1:2], float(HD * EPS)), reads=[C["epsb"].b], writes=[C["epsb"].b])
    for name, src, scale in [("gmix", "g_mix", 32.0), ("gffn", "g_ffn", 32.0)]:
        t = C[name]
        P.dma("sync", lambda e, t=t, src=src: e.dma_start(out=t[:], in_=bcast_rows(I[src], D)), writes=[t.b])
        P.op(V_, lambda e, t=t, scale=scale: e.tensor_scalar(out=t[:], in0=t[:], scalar1=scale, scalar2=None, op0=ALU.mult),
             reads=[t.b], writes=[t.b])
    for name, src, scale in [("gqa", "q_norm_a", 1.0), ("gka", "k_norm_a", 8.0), ("gqb", "q_norm_b", 1.0), ("gkb", "k_norm_b", 8.0)]:
        t = C[name]
        P.dma("sync", lambda e, t=t, src=src: e.dma_start(
            out=t.v("p (h d) -> p h d", d=HD), in_=bass.AP(I[src].tensor, 0, [[0, 128], [0, NH], [1, HD]])), writes=[t.b])
        if scale != 1.0:
            P.op(V_, lambda e, t=t, scale=scale: e.tensor_scalar(out=t[:], in0=t[:], scalar1=scale, scalar2=None, op0=ALU.mult),
                 reads=[t.b], writes=[t.b])
    for name, src, n in [("bf", "b_f", NH), ("wm", "wmeta", 256), ("hv", "hvalid", 4), ("brg", "b_rg", 4), ("bre", "b_re", 32)]:
        t = C[name]
        P.dma("sync", lambda e, t=t, src=src, n=n: e.dma_start(out=t[:], in_=bcast_rows(I[src], n)), writes=[t.b])


def rsqrt_act(k, out, ss, n):
    P, epsb = k.P, k.C["epsb"]
    col = {1024: 0, 64: 1}[n]
    P.op(S_, lambda e: e.activation(out=out[:], in_=ss[:], func=AF.Ln, bias=epsb[:, col:col + 1], scale=1.0),
         reads=[ss.b, epsb.b], writes=[out.b])
    P.op(S_, lambda e: e.activation(out=out[:], in_=out[:], func=AF.Exp, scale=-0.5), reads=[out.b], writes=[out.b])


def get(k, name, cols, dt=F32, parts=128):
    if name not in k.pbuf:
        k.pbuf[name] = k.A.alloc(name, cols, dt, parts)
    return k.pbuf[name]


def gtmp(k, name, cols, dt=F32, parts=128):
    return get(k, name + getattr(k, "tag", ""), cols, dt, parts)


def rms_tile(k, X, gain, tr, tag=""):
    P, C = k.P, k.C
    junk = gtmp(k, "junk", D, BF16)
    ss = gtmp(k, "ss", 1)
    rs = gtmp(k, "rs", 1)
    H = gtmp(k, "hbf", D, BF16)
    HT = get(k, "hT" + tag, D, BF16)
    P.op(S_, lambda e: e.activation(out=junk[:], in_=X[:], func=AF.Square, accum_out=ss[:, 0:1]), reads=[X.b], writes=[junk.b, ss.b])
    rsqrt_act(k, rs, ss, D)
    P.op(V_, lambda e: e.scalar_tensor_tensor(out=H[:], in0=X[:], scalar=rs[:, 0:1], in1=gain[:], op0=ALU.mult, op1=ALU.mult),
         reads=[X.b, rs.b, gain.b], writes=[H.b])
    ident = C["ident"]
    for kc in range(8):
        P.op(PE_, lambda e, kc=kc: e.transpose(tr[:, kc * 128:(kc + 1) * 128], H[:, kc * 128:(kc + 1) * 128], ident[:]),
             reads=[H.b, ident.b], writes=[tr.b])
    P.op(S_, lambda e: e.copy(out=HT[:], in_=tr[:, 0:D]), reads=[tr.b], writes=[HT.b])
    return HT


def proj(k, dst, dst_ap, HT, W, w_ap_fn):
    P = k.P
    for kc in range(8):
        P.op(PE_, lambda e, kc=kc: e.matmul(dst_ap, lhsT=HT[:, kc * 128:(kc + 1) * 128], rhs=w_ap_fn(kc), start=(kc == 0), stop=(kc == 7)),
             reads=[HT.b, W.b], writes=[dst.b])


def headnorm(k, src, src_ap, gain, out):
    P = k.P
    sq = gtmp(k, "sq", 512)
    ssk = gtmp(k, "ssk", NH)
    rstdk = gtmp(k, "rstdk", NH)
    t1 = gtmp(k, "t1", 512)
    P.op(S_, lambda e: e.activation(out=sq[:], in_=src_ap, func=AF.Square), reads=[src.b], writes=[sq.b])
    P.op(V_, lambda e: e.tensor_reduce(out=ssk[:], in_=sq.v("p (h d) -> p h d", d=HD), axis=AX.X, op=ALU.add), reads=[sq.b], writes=[ssk.b])
    rsqrt_act(k, rstdk, ssk, HD)
    P.op(V_, lambda e: e.tensor_tensor(out=t1.v("p (h d) -> p h d", d=HD), in0=src_ap.rearrange("p (h d) -> p h d", d=HD),
                                       in1=rstdk[:].unsqueeze(2).to_broadcast([128, NH, HD]), op=ALU.mult),
         reads=[src.b, rstdk.b], writes=[t1.b])
    P.op(G_, lambda e: e.tensor_tensor(out=out[:], in0=t1[:], in1=gain[:], op=ALU.mult), reads=[t1.b, gain.b], writes=[out.b])


def logf_from(k, fsrc, f_ap, LF):
    P, bf = k.P, k.C["bf"]
    z, za, ze, zl, zm = (gtmp(k, n, NH) for n in ["z", "za", "ze", "zl", "zm"])
    P.op(V_, lambda e: e.tensor_tensor(out=z[:], in0=f_ap, in1=bf[:], op=ALU.add), reads=[fsrc.b, bf.b], writes=[z.b])
    P.op(V_, lambda e: e.scalar_tensor_tensor(out=za[:], in0=z[:], scalar=-1.0, in1=z[:], op0=ALU.mult, op1=ALU.max), reads=[z.b], writes=[za.b])
    P.op(S_, lambda e: e.activation(out=ze[:], in_=za[:], func=AF.Exp, scale=-1.0), reads=[za.b], writes=[ze.b])
    P.op(S_, lambda e: e.activation(out=zl[:], in_=ze[:], func=AF.Ln, bias=1.0), reads=[ze.b], writes=[zl.b])
    P.op(V_, lambda e: e.tensor_single_scalar(out=zm[:], in_=z[:], scalar=0.0, op=ALU.min), reads=[z.b], writes=[zm.b])
    P.op(V_, lambda e: e.tensor_sub(out=LF[:], in0=zm[:], in1=zl[:]), reads=[zm.b, zl.b], writes=[LF.b])


def cumsum_tile(k, LF, Umat, carry, sm, csb, update_carry):
    P, ones = k.P, k.C["ones"]
    P.op(PE_, lambda e: e.matmul(sm[:, 8:16], lhsT=Umat[:], rhs=LF[:], start=True, stop=True), reads=[Umat.b, LF.b], writes=[sm.b])
    if update_carry:
        P.op(PE_, lambda e: e.matmul(sm[:, 16:24], lhsT=ones[:], rhs=LF[:], start=True, stop=True), reads=[ones.b, LF.b], writes=[sm.b])
    P.op(V_, lambda e: e.tensor_tensor(out=csb[:], in0=sm[:, 8:16], in1=carry[:], op=ALU.add), reads=[sm.b, carry.b], writes=[csb.b])
    if update_carry:
        P.op(V_, lambda e: e.tensor_tensor(out=carry[:], in0=sm[:, 16:24], in1=carry[:], op=ALU.add), reads=[sm.b, carry.b], writes=[carry.b])


def split3(k, csb, ST, c0, sign):
    P = k.P
    r1, r2 = gtmp(k, "r1", NH), gtmp(k, "r2", NH)
    v = ST.v("p (h c) -> p h c", c=70)
    P.op(V_, lambda e: e.tensor_scalar(out=v[:, :, c0], in0=csb[:], scalar1=sign, scalar2=None, op0=ALU.mult), reads=[csb.b], writes=[ST.b])
    P.op(V_, lambda e: e.scalar_tensor_tensor(out=r1[:], in0=csb[:], scalar=sign, in1=v[:, :, c0], op0=ALU.mult, op1=ALU.subtract),
         reads=[csb.b, ST.b], writes=[r1.b])
    P.op(V_, lambda e: e.tensor_copy(out=v[:, :, c0 + 1], in_=r1[:]), reads=[r1.b], writes=[ST.b])
    P.op(V_, lambda e: e.tensor_sub(out=r2[:], in0=r1[:], in1=v[:, :, c0 + 1]), reads=[r1.b, ST.b], writes=[r2.b])
    P.op(V_, lambda e: e.tensor_copy(out=v[:, :, c0 + 2], in_=r2[:]), reads=[r2.b], writes=[ST.b])


def aug_transpose(k, ST, ktp, OUT):
    P, ident = k.P, k.C["ident"]
    v = ST.v("p (h c) -> p h c", c=70)
    for h in range(NH):
        P.op(PE_, lambda e, h=h: e.transpose(ktp[0:70, h * 128:(h + 1) * 128], v[:, h, :], ident[:]), reads=[ST.b, ident.b], writes=[ktp.b])
    P.op(S_, lambda e: e.copy(out=OUT[:], in_=ktp[0:70, 0:NH * 128]), reads=[ktp.b], writes=[OUT.b])


def phase_full(k):
    P, C, I, O, S = k.P, k.C, k.I, k.O, k.S
    Wkvf = get(k, "Wkvf", 8 * 1032, BF16)
    Wv = Wkvf.v("p (k c) -> p k c", c=1032)
    for kc in range(8):
        P.dma(G_, lambda e, kc=kc: e.dma_start(out=Wv[:, kc, :], in_=I["w_in"][kc * 128:(kc + 1) * 128, C_KB:C_KB + 1032]), writes=[Wkvf.b])
    tr = pbank(k, "tr", 0, 1, BF16)
    kbp = [pbank(k, "kbp%d" % i, 1 + i) for i in range(2)]
    vbp = [pbank(k, "vbp%d" % i, 3 + i) for i in range(2)]
    sm = pbank(k, "sm", 5)
    offp = pbank(k, "offp", 6)
    ktp = pbank(k, "ktp", 7, 1, BF16)
    xin = [get(k, "xin%d" % i, D) for i in range(2)]
    kout = [get(k, "kout%d" % i, 512) for i in range(2)]
    vout = [get(k, "vout%d" % i, 512) for i in range(2)]
    kst = [get(k, "kst%d" % i, NH * 70, BF16) for i in range(2)]
    vst = [get(k, "vst%d" % i, NH * 65, BF16) for i in range(2)]
    ktT = [get(k, "ktT%d" % i, NH * 128, BF16, parts=70) for i in range(2)]
    lf = [get(k, "lf%d" % i, NH) for i in range(2)]
    lfw2 = [get(k, "lfw%d" % i, 32) for i in range(2)]
    carry = get(k, "carry", NH)
    csb2 = [get(k, "csb%d" % i, NH) for i in range(2)]
    for i in range(2):
        P.op(G_, lambda e, i=i: e.memset(kst[i][:], 1.0), writes=[kst[i].b])
        P.op(G_, lambda e, i=i: e.memset(vst[i][:], 1.0), writes=[vst[i].b])
    cnt = [0]

    lcnt = [0]

    def kv_loads(mode, src, r0):
        i = lcnt[0] % 2
        lcnt[0] += 1
        if mode == "x":
            X = xin[i]
            P.dma("sync", lambda e: e.dma_start(out=X[:], in_=src[r0:r0 + 128, :]), writes=[X.b])
        else:
            ck, cv, cl = src
            KO, VO, LF = kout[i], vout[i], lf[i]
            P.dma("sync", lambda e: e.dma_start(out=KO[:], in_=ck[r0:r0 + 128, :]), writes=[KO.b])
            P.dma("sync", lambda e: e.dma_start(out=VO[:], in_=cv[r0:r0 + 128, :]), writes=[VO.b])
            P.dma("sync", lambda e: e.dma_start(out=LF[:], in_=cl[r0:r0 + 128, :]), writes=[LF.b])

    def kv_tile(mode, src, r0, dst_k, dst_v, dst_lf, kt_dsts, vs_dsts, Umat, carry_t, upd, wm_ft=None, first=False, last=False):
        i = cnt[0] % 2
        cnt[0] += 1
        k.tag = "_p%d" % i
        lfw, csb = lfw2[i], csb2[i]
        KO, VO, KS, VSb, KTT, LF = kout[i], vout[i], kst[i], vst[i], ktT[i], lf[i]
        if mode == "x":
            X = xin[i]
            HT = rms_tile(k, X, C["gmix"], tr, tag=str(i))
            proj(k, kbp[i], kbp[i][:, 0:512], HT, Wkvf, lambda kc: Wv[:, kc, 0:512])
            proj(k, vbp[i], vbp[i][:, 0:512], HT, Wkvf, lambda kc: Wv[:, kc, 512:1024])
            proj(k, sm, sm[:, 0:8], HT, Wkvf, lambda kc: Wv[:, kc, 1024:1032])
            headnorm(k, kbp[i], kbp[i][:, 0:512], C["gkb"], KO)
            P.op(S_, lambda e: e.copy(out=VO[:], in_=vbp[i][:, 0:512]), reads=[vbp[i].b], writes=[VO.b])
            logf_from(k, sm, sm[:, 0:8], LF)
            P.dma("sync", lambda e: e.dma_start(out=dst_k, in_=KO[:]), reads=[KO.b])
            P.dma("sync", lambda e: e.dma_start(out=dst_v, in_=VO[:]), reads=[VO.b])
            P.dma("sync", lambda e: e.dma_start(out=dst_lf, in_=LF[:]), reads=[LF.b])
        P.op(G_, lambda e: e.tensor_copy(out=KS.v("p (h c) -> p h c", c=70)[:, :, 0:HD], in_=KO.v("p (h d) -> p h d", d=HD)),
             reads=[KO.b], writes=[KS.b])
        P.op(G_, lambda e: e.tensor_copy(out=VSb.v("p (h c) -> p h c", c=65)[:, :, 0:HD], in_=VO.v("p (h d) -> p h d", d=HD)),
             reads=[VO.b], writes=[VSb.b])
        P.op(V_, lambda e: e.memset(VSb.v("p (h c) -> p h c", c=65)[:, :, 64:65], 1.0), reads=[VSb.b], writes=[VSb.b])
        P.op(V_, lambda e: e.memset(KS.v("p (h c) -> p h c", c=70)[:, :, 64:67], 1.0), reads=[KS.b], writes=[KS.b])
        for (dst, p0, p1) in vs_dsts:
            P.dma("sync", lambda e, dst=dst, p0=p0, p1=p1: e.dma_start(out=dst, in_=VSb[p0:p1, :]), reads=[VSb.b])
        cumsum_tile(k, LF, Umat, carry_t, sm, csb, upd)
        if wm_ft is not None:
            P.op(V_, lambda e: e.tensor_tensor(out=lfw.v("p (a h) -> p a h", h=NH), in0=LF[:].unsqueeze(1).to_broadcast([128, 4, NH]),
                                               in1=C["wm"][:, wm_ft * 4:(wm_ft + 1) * 4].unsqueeze(2).to_broadcast([128, 4, NH]), op=ALU.mult),
                 reads=[LF.b, C["wm"].b], writes=[lfw.b])
            P.op(PE_, lambda e: e.matmul(offp[:, 0:32], lhsT=C["ones"][:], rhs=lfw[:], start=first, stop=last),
                 reads=[C["ones"].b, lfw.b], writes=[offp.b])
        split3(k, csb, KS, 67, -1.0)
        aug_transpose(k, KS, ktp, KTT)
        KTv = KTT.v("p (h t) -> p h t", t=128)
        for (dst, c0, c1) in kt_dsts:
            P.dma("sync", lambda e, dst=dst, c0=c0, c1=c1: e.dma_start(out=dst, in_=KTv[:, :, c0:c1]), reads=[KTT.b])

    NT = SEQ // 128
    tiles = []
    for ft in range(NT):
        r0 = ft * 128
        tiles.append(dict(pre=("zero" if ft == 0 else None), mode="x", src=I["xfull"], r0=r0,
                          args=(O["o_bk"][r0:r0 + 128, :], O["o_bv"][r0:r0 + 128, :], O["o_blf"][r0:r0 + 128, :],
                                [(S["KT"][:, :, r0:r0 + 128].rearrange("h p t -> p h t"), 0, 128)], [(S["VS"][r0:r0 + 128, :], 0, 128)],
                                C["U"], carry, True), kw=dict(wm_ft=ft, first=(ft == 0), last=(ft == NT - 1)),
                          post=("offs" if ft == NT - 1 else None)))
    for sbi in range(2):
        for ct in range(32):
            r0 = ct * 128
            tiles.append(dict(pre=("zero" if ct == 0 else None), mode="cache", src=(I["cbk"][sbi], I["cbv"][sbi], I["cblf"][sbi]), r0=r0,
                              args=(None, None, None, [(S["KTS"][sbi, :, :, r0:r0 + 128].rearrange("h p t -> p h t"), 0, 128)],
                                    [(S["VSS"][sbi, r0:r0 + 128, :], 0, 128)], C["U"], carry, True), kw={},
                              post=(("c2", sbi) if ct == 31 else None)))
    tiles.append(dict(pre=None, mode="x", src=I["xs"], r0=0,
                      args=(O["o_bks"][:, :], O["o_bvs"][:, :], O["o_blfs"][:, :],
                            [(S["KTS"][sbi, :, :, 4096:4160].rearrange("h p t -> p h t"), sbi * 64, sbi * 64 + 64) for sbi in range(2)],
                            [(S["VSS"][sbi, 4096:4160, :], sbi * 64, sbi * 64 + 64) for sbi in range(2)], C["U2"], C["carry2"], False),
                      kw={}, post=None))
    kv_loads(tiles[0]["mode"], tiles[0]["src"], tiles[0]["r0"])
    for ti, td in enumerate(tiles):
        if ti + 1 < len(tiles):
            nx = tiles[ti + 1]
            kv_loads(nx["mode"], nx["src"], nx["r0"])
        if td["pre"] == "zero":
            P.op(V_, lambda e: e.memset(carry[:], 0.0), reads=[carry.b], writes=[carry.b])
        kv_tile(td["mode"], td["src"], td["r0"], *td["args"], **td["kw"])
        if td["post"] == "offs":
            P.op(V_, lambda e: e.tensor_copy(out=C["offs"][:], in_=offp[:, 0:32]), reads=[offp.b], writes=[C["offs"].b])
        elif td["post"] is not None:
            sbi = td["post"][1]
            P.op(V_, lambda e, sbi=sbi: e.tensor_copy(out=C["carry2"][sbi * 64:(sbi + 1) * 64, :], in_=carry[sbi * 64:(sbi + 1) * 64, :]),
                 reads=[carry.b], writes=[C["carry2"].b])

def slot(s):
    return (s // 6) * 512 + (s % 6) * 80


def phase_own(k):
    k.tag = ""
    P, C, I, O, S, A = k.P, k.C, k.I, k.O, k.S, k.A
    I32 = mybir.dt.int32
    Wq = get(k, "Wq", 8 * 2056, BF16)
    Wv = Wq.v("p (k c) -> p k c", c=2056)
    for kc in range(8):
        P.dma(G_, lambda e, kc=kc: e.dma_start(out=Wv[:, kc, 0:2048], in_=I["w_in"][kc * 128:(kc + 1) * 128, 0:2048]), writes=[Wq.b])
        P.dma(G_, lambda e, kc=kc: e.dma_start(out=Wv[:, kc, 2048:2056], in_=I["w_in"][kc * 128:(kc + 1) * 128, C_F:C_F + 8]), writes=[Wq.b])
    BT = get(k, "BT", NH * 640, BF16)
    BTv = BT.v("p (h c) -> p h c", c=640)
    ext = get(k, "ext", 896, F32, parts=8)
    Xt = get(k, "Xt", 128)
    Jm = get(k, "Jm", 128)
    ps0 = pbank(k, "ps0", 0)
    P.dma("sync", lambda e: e.dma_start(out=ext[:, 0:384], in_=I["rel_bias"][:, 129:513]), writes=[ext.b])
    P.op(V_, lambda e: e.tensor_copy(out=ext[:, 384:896], in_=ext[:, 383:384].to_broadcast([8, 512])), reads=[ext.b], writes=[ext.b])
    P.dma("sync", lambda e: e.dma_start(out=S["EXT"][:, :], in_=ext[:]), reads=[ext.b], writes=[BT.b])
    P.op(G_, lambda e: e.memset(Jm[:], 0.0), writes=[Jm.b])
    P.op(G_, lambda e: e.affine_select(out=Jm[:], in_=Jm[:], pattern=[[1, 128]], compare_op=ALU.not_equal, fill=1.0,
                                       base=-127, channel_multiplier=1), reads=[Jm.b], writes=[Jm.b])
    for h in range(NH):
        for t in range(5):
            P.dma("sync", lambda e, h=h, t=t: e.dma_start(out=Xt[:], in_=bass.AP(S["EXT"].tensor, h * 896 + 512 - 128 * t, [[1, 128], [1, 128]])),
                  reads=[BT.b], writes=[Xt.b])
            P.op(PE_, lambda e: e.matmul(ps0[:, 0:128], lhsT=Jm[:], rhs=Xt[:], start=True, stop=True), reads=[Jm.b, Xt.b], writes=[ps0.b])
            P.op(V_, lambda e, h=h, t=t: e.tensor_copy(out=BTv[:, h, t * 128:(t + 1) * 128], in_=ps0[:, 0:128]), reads=[ps0.b], writes=[BT.b])
    P.op(V_, lambda e: e.memset(BTv[0:64, :, 64:128], -30000.0), reads=[BT.b], writes=[BT.b])
    P.op(V_, lambda e: e.memset(BTv[64:128, :, 512:576], -30000.0), reads=[BT.b], writes=[BT.b])
    M = get(k, "M", 16 * 512, BF16)
    Mv = M.v("p (c t) -> p c t", t=512)
    qrow = get(k, "qrow", 512)
    kpi = T(get(k, "kpi", 16).ap.bitcast(I32), "kpi")
    kpf = get(k, "kpf", 16)
    P.dma("sync", lambda e: e.dma_start(out=qrow[:], in_=bcast_rows(I["qpos"], 512)), writes=[qrow.b])
    P.op(G_, lambda e: e.iota(kpi[:], pattern=[[128, 16]], base=0, channel_multiplier=1), writes=[kpi.b])
    P.op(V_, lambda e: e.tensor_copy(out=kpf[:], in_=kpi[:]), reads=[kpi.b], writes=[kpf.b])
    for c in range(16):
        P.op(V_, lambda e, c=c: e.tensor_scalar(out=Mv[:, c, :], in0=qrow[:], scalar1=kpf[:, c:c + 1], scalar2=None, op0=ALU.is_ge),
             reads=[qrow.b, kpf.b], writes=[M.b])
        P.op(V_, lambda e, c=c: e.tensor_scalar(out=Mv[:, c, :], in0=Mv[:, c, :], scalar1=30000.0, scalar2=-30000.0, op0=ALU.mult, op1=ALU.add),
             reads=[M.b], writes=[M.b])
    Uadd = get(k, "Uadd", 128, BF16)
    P.op(V_, lambda e: e.tensor_scalar(out=Uadd[:], in0=C["U"][:], scalar1=30000.0, scalar2=-30000.0, op0=ALU.mult, op1=ALU.add),
         reads=[C["U"].b], writes=[Uadd.b])
    Sm = [get(k, "Sm%d" % i, 512) for i in range(2)]

    qaT = get(k, "qaT", 4 * 512, BF16)
    kaT = get(k, "kaT", 4 * 1024, BF16)
    va = [get(k, "va%d" % i, NH * 65, BF16) for i in range(8)]
    qbT = get(k, "qbT", NH * 512, BF16, parts=70)
    qst = get(k, "qst", NH * 70, BF16)
    stag = get(k, "stag", 512, BF16)
    xin = [get(k, "xin%d" % i, D) for i in range(2)]
    fo = [get(k, "fo%d" % i, 512) for i in range(3)]
    lf = get(k, "lf", NH)
    csb = get(k, "csb", NH)
    carryq = get(k, "carryq", NH)
    ost = [get(k, "ost%d" % i, 1024, BF16) for i in range(4)]
    Sb = get(k, "Sb", 640)
    Pb = get(k, "Pb", 640, BF16)
    Pt = [get(k, "Pt%d" % i, 512, BF16) for i in range(2)]
    ktc = [get(k, "ktc%d" % i, NH * 512, BF16, parts=70) for i in range(2)]
    vc = [get(k, "vc%d" % i, 4 * 520, BF16) for i in range(2)]
    rc = get(k, "rc", 1)
    zb = get(k, "zb", 512, BF16)
    P.op(G_, lambda e: e.memset(zb[:], 0.0), writes=[zb.b])
    qaTv = qaT.v("p (g t) -> p g t", t=512)
    kaTv = kaT.v("p (g t) -> p g t", t=1024)
    qbTv = qbT.v("p (h t) -> p h t", t=512)
    P.op(G_, lambda e: e.memset(qst[:], 1.0), writes=[qst.b])
    xcnt = [0]

    def psum_proj():
        return dict(tr=pbank(k, "tr", 0, 1, BF16), pj=[pbank(k, "pj%d" % i, 1 + i) for i in range(2)], sm=pbank(k, "sm", 3),
                    ktp=pbank(k, "ktp", 4, 1, BF16), n=[0])

    def next_pj(pp):
        pp["n"][0] += 1
        return pp["pj"][pp["n"][0] % 2]

    def load_x(src, r0):
        X = xin[xcnt[0] % 2]
        xcnt[0] += 1
        P.dma("sync", lambda e: e.dma_start(out=X[:], in_=src[r0:r0 + 128, :]), writes=[X.b])
        return X

    def to_T4(pp, src_f32, dstv, c0, ncols=128, src_c0=0):
        P.op(G_, lambda e: e.tensor_copy(out=stag[:], in_=src_f32[:]), reads=[src_f32.b], writes=[stag.b])
        tr = pp["tr"]
        for g in range(4):
            P.op(PE_, lambda e, g=g: e.transpose(tr[:, g * 128:(g + 1) * 128], stag[:, g * 128:(g + 1) * 128], C["ident"][:]),
                 reads=[stag.b, C["ident"].b], writes=[tr.b])
        return tr

    def ka_va(pp, HT, kt_idx, dst, vcol_fn, out_k=None, out_v=None):
        pj = next_pj(pp)
        proj(k, pj, pj[:, 0:512], HT, Wq, lambda kc: Wv[:, kc, C_KA:C_KA + 512])
        KAO = fo[0]
        headnorm(k, pj, pj[:, 0:512], C["gka"], KAO)
        if out_k is not None:
            P.dma("sync", lambda e: e.dma_start(out=out_k, in_=KAO[:]), reads=[KAO.b])
        tr = to_T4(pp, KAO, None, 0)
        P.op(S_, lambda e: e.copy(out=dst[0][:, :, dst[1]:dst[1] + 128], in_=tr[:, 0:512].rearrange("p (g t) -> p g t", t=128)),
             reads=[tr.b], writes=[dst[2].b])
        pj2 = next_pj(pp)
        proj(k, pj2, pj2[:, 0:512], HT, Wq, lambda kc: Wv[:, kc, C_VA:C_VA + 512])
        VAO = fo[1]
        P.op(S_, lambda e: e.copy(out=VAO[:], in_=pj2[:, 0:512]), reads=[pj2.b], writes=[VAO.b])
        if out_v is not None:
            P.dma("sync", lambda e: e.dma_start(out=out_v, in_=VAO[:]), reads=[VAO.b])
        if kt_idx is not None:
            VA = va[kt_idx]
            vav = VA.v("p (h c) -> p h c", c=65)
            P.op(G_, lambda e: e.tensor_copy(out=vav[:, :, 0:HD], in_=VAO.v("p (h d) -> p h d", d=HD)), reads=[VAO.b], writes=[VA.b])
            vcol_fn(VA, vav)
        return KAO, VAO

    def ones_col(VA, vav):
        P.op(G_, lambda e: e.memset(vav[:, :, 64:65], 1.0), reads=[VA.b], writes=[VA.b])

    def q_side(pp, HT, Umat, carry_t, upd, qa_dst, qb_dst):
        pj = next_pj(pp)
        proj(k, pj, pj[:, 0:512], HT, Wq, lambda kc: Wv[:, kc, C_QA:C_QA + 512])
        QAO = fo[2]
        headnorm(k, pj, pj[:, 0:512], C["gqa"], QAO)
        tr = to_T4(pp, QAO, None, 0)
        P.op(S_, lambda e: e.copy(out=qa_dst[0], in_=tr[:, 0:512].rearrange("p (g t) -> p g t", t=128)), reads=[tr.b], writes=[qa_dst[1].b])
        pj2 = next_pj(pp)
        proj(k, pj2, pj2[:, 0:512], HT, Wq, lambda kc: Wv[:, kc, C_QB:C_QB + 512])
        QBO = fo[2]
        headnorm(k, pj2, pj2[:, 0:512], C["gqb"], QBO)
        P.op(G_, lambda e: e.tensor_copy(out=qst.v("p (h c) -> p h c", c=70)[:, :, 0:HD], in_=QBO.v("p (h d) -> p h d", d=HD)),
             reads=[QBO.b], writes=[qst.b])
        sm = pp["sm"]
        proj(k, sm, sm[:, 0:8], HT, Wq, lambda kc: Wv[:, kc, 2048:2056])
        logf_from(k, sm, sm[:, 0:8], lf)
        cumsum_tile(k, lf, Umat, carry_t, sm, csb, upd)
        split3(k, csb, qst, 64, 1.0)
        ktp = pp["ktp"]
        qv = qst.v("p (h c) -> p h c", c=70)
        for h in range(NH):
            P.op(PE_, lambda e, h=h: e.transpose(ktp[0:70, h * 128:(h + 1) * 128], qv[:, h, :], C["ident"][:]),
                 reads=[qst.b, C["ident"].b], writes=[ktp.b])
        P.op(S_, lambda e: e.copy(out=qb_dst[0], in_=ktp[0:70, 0:1024].rearrange("p (h t) -> p h t", t=128)), reads=[ktp.b], writes=[qb_dst[1].b])

    def band(nq, q_ap_fn, key_tiles, ost_t, last_rows):
        Sbp = [pbank(k, "Sbp%d" % i, 2 * i, 2) for i in range(2)]
        Obp = [pbank(k, "Obp%d" % i, 4 + 2 * i, 2) for i in range(2)]
        return Sbp, Obp

    def band_unit(Sbp, Obp, u, nq, h, q_ap, k_aps, v_tiles, ost_t, last_rows):
        sp, op_ = Sbp[u % 2], Obp[(u // 8) % 2]
        nk = len(k_aps)
        for t in range(nk):
            rows = last_rows if t == nk - 1 else 128
            P.op(PE_, lambda e, t=t, rows=rows: e.matmul(sp[0:rows, t * 128:t * 128 + nq], lhsT=k_aps[t][0], rhs=q_ap, start=True, stop=True),
                 reads=[k_aps[t][1].b, qaT.b], writes=[sp.b])
        w = (nk - 1) * 128 + nq
        BTh = BTv[:, h, :]
        if nq == 128 and last_rows == 128:
            P.op(V_, lambda e: e.tensor_tensor(out=Sb[:, 0:640], in0=sp[:, 0:640], in1=BTh, op=ALU.add), reads=[sp.b, BT.b], writes=[Sb.b])
            P.op(S_, lambda e: e.activation(out=Pb[:, 0:640], in_=Sb[:, 0:640], func=AF.Exp), reads=[Sb.b], writes=[Pb.b])
        else:
            for t in range(nk):
                rows = last_rows if t == nk - 1 else 128
                P.op(V_, lambda e, t=t, rows=rows: e.tensor_tensor(out=Sb[0:rows, t * 128:t * 128 + nq], in0=sp[0:rows, t * 128:t * 128 + nq],
                                                                   in1=BTv[0:rows, h, t * 128:t * 128 + nq], op=ALU.add),
                     reads=[sp.b, BT.b], writes=[Sb.b])
                P.op(S_, lambda e, t=t, rows=rows: e.activation(out=Pb[0:rows, t * 128:t * 128 + nq], in_=Sb[0:rows, t * 128:t * 128 + nq], func=AF.Exp),
                     reads=[Sb.b], writes=[Pb.b])
        o0 = slot(h)
        for t in range(nk):
            rows = last_rows if t == nk - 1 else 128
            VA = v_tiles[t]
            P.op(PE_, lambda e, t=t, rows=rows, VA=VA: e.matmul(op_[0:nq, o0:o0 + 65], lhsT=Pb[0:rows, t * 128:t * 128 + nq],
                                                               rhs=VA.v("p (h c) -> p h c", c=65)[0:rows, h, :], start=(t == 0), stop=(t == nk - 1)),
                 reads=[Pb.b, VA.b], writes=[op_.b])
        P.op(V_, lambda e: e.reciprocal(out=rc[0:nq, :], in_=op_[0:nq, o0 + 64:o0 + 65]), reads=[op_.b], writes=[rc.b])
        P.op(V_, lambda e: e.tensor_scalar(out=ost_t[0:nq, h * 64:(h + 1) * 64], in0=op_[0:nq, o0:o0 + 64], scalar1=rc[0:nq, 0:1], scalar2=None, op0=ALU.mult),
             reads=[op_.b, rc.b], writes=[ost_t.b])

    def fox(nq, q_ap_fn, chunks, ost_list):
        Sp = [pbank(k, "Sp%d" % i, i) for i in range(2)]
        Op = pbank(k, "Op", 2, 6)
        nqs = (nq + 127) // 128
        qn = min(nq, 128)
        units = []

        def load(ci):
            (kt_src, v_src, nkeys, mask_fn) = chunks[ci]
            KC, VC = ktc[ci % 2], vc[ci % 2]
            KCv = KC.v("p (h t) -> p h t", t=512)
            VCv = VC.v("p (a c) -> p a c", c=520)
            P.dma("sync", lambda e: e.dma_start(out=KCv[:, :, 0:nkeys], in_=kt_src), writes=[KC.b])
            ntile = (nkeys + 127) // 128
            if nkeys >= 128:
                P.dma("sync", lambda e: e.dma_start(out=VCv[:, 0:ntile, :], in_=v_src.rearrange("(a p) c -> p a c", p=128)), writes=[VC.b])
            else:
                P.dma("sync", lambda e: e.dma_start(out=VCv[0:nkeys, 0, :], in_=v_src), writes=[VC.b])

        for ci, (kt_src, v_src, nkeys, mask_fn) in enumerate(chunks):
            KC, VC = ktc[ci % 2], vc[ci % 2]
            KCv = KC.v("p (h t) -> p h t", t=512)
            VCv = VC.v("p (a c) -> p a c", c=520)
            ntile = (nkeys + 127) // 128
            for kt in range(ntile):
                rows = min(128, nkeys - kt * 128)
                for h in range(NH):
                    units.append((ci, kt, h, rows, KC, KCv, VC, VCv, mask_fn(kt) if mask_fn else None))
        load(0)
        for bk in range(6):
            P.op(PE_, lambda e, bk=bk: e.matmul(Op[:, bk * 512:(bk + 1) * 512], lhsT=zb[:, 0:128], rhs=zb[:, 0:512], start=True, stop=True),
                 reads=[zb.b], writes=[Op.b])
        nu = len(units)
        first = {}
        last = {}
        for ui, u in enumerate(units):
            first.setdefault(u[2], ui)
            last[u[2]] = ui

        def pv(ui):
            (ci, kt, h, rows, KC, KCv, VC, VCv, mk) = units[ui]
            PT = Pt[ui % 2]
            for qs in range(nqs):
                o0 = slot(h * nqs + qs)
                P.op(PE_, lambda e, qs=qs, o0=o0: e.matmul(Op[0:qn, o0:o0 + 65], lhsT=PT[0:rows, qs * 128:qs * 128 + qn], rhs=VCv[0:rows, kt, h * 65:(h + 1) * 65],
                                                           start=False, stop=(ui == last[h])), reads=[PT.b, VC.b], writes=[Op.b])

        for ui, (ci, kt, h, rows, KC, KCv, VC, VCv, mk) in enumerate(units):
            SP, PT = Sp[ui % 2], Pt[ui % 2]
            P.op(PE_, lambda e, SP=SP, KCv=KCv, rows=rows, kt=kt, h=h, mk=mk: e.matmul(SP[0:rows, 0:nq], lhsT=KCv[:, h, kt * 128:kt * 128 + rows], rhs=q_ap_fn(h),
                                                                                      start=True, stop=(mk is None)), reads=[KC.b, qbT.b], writes=[SP.b])
            if mk is not None:
                P.op(PE_, lambda e, SP=SP, rows=rows, mk=mk: e.matmul(SP[0:rows, 0:nq], lhsT=C["ident"][0:rows, 0:rows], rhs=mk[0][0:rows, 0:nq],
                                                                      start=False, stop=True), reads=[C["ident"].b, mk[1].b], writes=[SP.b])
            P.op(S_, lambda e, SP=SP, PT=PT, rows=rows: e.activation(out=PT[0:rows, 0:nq], in_=SP[0:rows, 0:nq], func=AF.Exp), reads=[SP.b], writes=[PT.b])
            if ui >= 1:
                pv(ui - 1)
            if (ui == 0 or units[ui - 1][0] != ci) and ci + 1 < len(chunks):
                load(ci + 1)
        pv(nu - 1)
        for h in range(NH):
            for qs in range(nqs):
                o0 = slot(h * nqs + qs)
                ot = ost_list[qs]
                P.op(V_, lambda e, o0=o0: e.reciprocal(out=rc[0:qn, :], in_=Op[0:qn, o0 + 64:o0 + 65]), reads=[Op.b], writes=[rc.b])
                P.op(V_, lambda e, o0=o0, ot=ot, h=h: e.tensor_scalar(out=ot[0:qn, 512 + h * 64:512 + (h + 1) * 64], in0=Op[0:qn, o0:o0 + 64],
                                                                      scalar1=rc[0:qn, 0:1], scalar2=None, op0=ALU.mult), reads=[Op.b, rc.b], writes=[ot.b])

    for n in range(4):
        pp = psum_proj()
        for t in range(4):
            X = load_x(I["xhalo"], n * 512 + t * 128)
            HT = rms_tile(k, X, C["gmix"], pp["tr"])

            def hv_col(VA, vav, n=n):
                P.op(V_, lambda e: e.tensor_copy(out=vav[:, :, 64:65], in_=C["hv"][:, n:n + 1].unsqueeze(1).to_broadcast([128, NH, 1])),
                     reads=[VA.b, C["hv"].b], writes=[VA.b])
            ka_va(pp, HT, t, (kaTv, t * 128, kaT), hv_col)
        P.op(V_, lambda e, n=n: e.tensor_copy(out=carryq[:], in_=C["offs"][:, n * 8:(n + 1) * 8]), reads=[C["offs"].b, carryq.b], writes=[carryq.b])
        for t in range(4):
            r0 = n * 512 + t * 128
            X = load_x(I["xown"], r0)
            HT = rms_tile(k, X, C["gmix"], pp["tr"])
            ka_va(pp, HT, 4 + t, (kaTv, (4 + t) * 128, kaT), ones_col,
                  out_k=(O["o_ak"][t * 128:(t + 1) * 128, :] if n == 3 else None), out_v=(O["o_av"][t * 128:(t + 1) * 128, :] if n == 3 else None))
            q_side(pp, HT, C["U"], carryq, True, (qaTv[:, :, t * 128:(t + 1) * 128], qaT), (qbTv[:, :, t * 128:(t + 1) * 128], qbT))
        if n == 0 and os.environ.get("KNOROUTER"):
            P.dma(G_, lambda e: e.dma_start(out=O["o_qb"][:, :], in_=qbT[:]), reads=[qbT.b])
            P.dma(G_, lambda e: e.dma_start(out=O["o_kt"][:, :, :], in_=S["KT"][:, :, 0:256]))
            P.dma(G_, lambda e: e.dma_start(out=O["o_vs"][:, :], in_=S["VS"][0:256, :]))
        P.barrier()
        Sbp, Obp = band(0, None, None, None, 0)
        u = 0
        for cp in range(4):
            for h in range(NH):
                g, r0 = h // 2, (h % 2) * 64
                k_aps = [(kaTv[r0:r0 + 64, g, (cp + t) * 128:(cp + t + 1) * 128], kaT) for t in range(5)]
                band_unit(Sbp, Obp, u, 128, h, qaTv[r0:r0 + 64, g, cp * 128:(cp + 1) * 128], k_aps, [va[cp + t] for t in range(5)], ost[cp], 128)
                u += 1
        P.barrier()
        chunks = []
        for kc in range(4 * n + 4):
            mf = None
            if kc >= 4 * n:
                mf = (lambda kt, kc=kc, n=n: (Mv[:, (kc - 4 * n) * 4 + kt, :], M))
            chunks.append((S["KT"][:, :, kc * 512:(kc + 1) * 512].rearrange("h p t -> p h t"), S["VS"][kc * 512:(kc + 1) * 512, :], 512, mf))
        fox(512, lambda h: qbTv[:, h, :], chunks, ost)
        for t in range(4):
            r0 = n * 512 + t * 128
            P.dma("sync", lambda e, t=t, r0=r0: e.dma_start(out=S["OT"][r0:r0 + 128, :], in_=ost[t][:]), reads=[ost[t].b])
        P.barrier()

    pp = psum_proj()
    X = load_x(I["xs"], 0)
    HT = rms_tile(k, X, C["gmix"], pp["tr"])
    kaTn = get(k, "kaTn", 4 * 128, BF16)
    kaTnv = kaTn.v("p (g t) -> p g t", t=128)
    KAO, VAO = ka_va(pp, HT, None, (kaTnv, 0, kaTn), None)
    KAN = get(k, "KAN", 512)
    VAN = get(k, "VAN", 512)
    P.op(V_, lambda e: e.tensor_copy(out=KAN[:], in_=KAO[:]), reads=[KAO.b], writes=[KAN.b])
    P.op(V_, lambda e: e.tensor_copy(out=VAN[:], in_=VAO[:]), reads=[VAO.b], writes=[VAN.b])
    qaTs = get(k, "qaTs", 4 * 128, BF16)
    qbTs = get(k, "qbTs", NH * 128, BF16, parts=70)
    qaTsv = qaTs.v("p (g t) -> p g t", t=128)
    qbTsv = qbTs.v("p (h t) -> p h t", t=128)
    q_side(pp, HT, C["U2"], C["carry2"], False, (qaTsv[:, :, :], qaTs), (qbTsv[:, :, :], qbTs))
    for sbi in range(2):
        p0 = sbi * 64
        P.dma("sync", lambda e, sbi=sbi: e.dma_start(out=O["o_aks"][sbi, 0:448, :], in_=I["cak"][sbi, 64:512, :]))
        P.dma("sync", lambda e, sbi=sbi: e.dma_start(out=O["o_avs"][sbi, 0:448, :], in_=I["cav"][sbi, 64:512, :]))
        P.dma("sync", lambda e, sbi=sbi, p0=p0: e.dma_start(out=O["o_aks"][sbi, 448:512, :], in_=KAN[p0:p0 + 64, :]), reads=[KAN.b])
        P.dma("sync", lambda e, sbi=sbi, p0=p0: e.dma_start(out=O["o_avs"][sbi, 448:512, :], in_=VAN[p0:p0 + 64, :]), reads=[VAN.b])
        for t in range(4):
            KC_ = fo[0]
            P.dma("sync", lambda e, sbi=sbi, t=t: e.dma_start(out=KC_[:], in_=I["cak"][sbi, t * 128:(t + 1) * 128, :]), writes=[KC_.b])
            tr = to_T4(pp, KC_, None, 0)
            P.op(S_, lambda e, t=t, tr=tr: e.copy(out=kaTv[:, :, t * 128:(t + 1) * 128], in_=tr[:, 0:512].rearrange("p (g t) -> p g t", t=128)),
                 reads=[tr.b], writes=[kaT.b])
            VC_ = fo[1]
            P.dma("sync", lambda e, sbi=sbi, t=t: e.dma_start(out=VC_[:], in_=I["cav"][sbi, t * 128:(t + 1) * 128, :]), writes=[VC_.b])
            VA = va[t]
            vav = VA.v("p (h c) -> p h c", c=65)
            P.op(G_, lambda e, vav=vav: e.tensor_copy(out=vav[:, :, 0:HD], in_=VC_.v("p (h d) -> p h d", d=HD)), reads=[VC_.b], writes=[VA.b])
            ones_col(VA, vav)
        P.op(V_, lambda e, p0=p0: e.tensor_copy(out=kaTv[:, :, 512:576], in_=kaTnv[:, :, p0:p0 + 64]), reads=[kaTn.b], writes=[kaT.b])
        VA = va[4]
        vav = VA.v("p (h c) -> p h c", c=65)
        P.op(V_, lambda e, p0=p0, vav=vav: e.tensor_copy(out=vav[0:64, :, 0:HD], in_=VAN.v("p (h d) -> p h d", d=HD)[p0:p0 + 64, :, :]),
             reads=[VAN.b], writes=[VA.b])
        ones_col(VA, vav)
        P.barrier()
        Sbp, Obp = band(0, None, None, None, 0)
        for h in range(NH):
            g, r0 = h // 2, (h % 2) * 64
            k_aps = [(kaTv[r0:r0 + 64, g, t * 128:t * 128 + (64 if t == 4 else 128)], kaT) for t in range(5)]
            band_unit(Sbp, Obp, h, 64, h, qaTsv[r0:r0 + 64, g, p0:p0 + 64], k_aps, [va[t] for t in range(5)], ost[0], 64)
        P.barrier()
        chunks = []
        for kc in range(8):
            chunks.append((S["KTS"][sbi, :, :, kc * 512:(kc + 1) * 512].rearrange("h p t -> p h t"), S["VSS"][sbi, kc * 512:(kc + 1) * 512, :], 512, None))
        chunks.append((S["KTS"][sbi, :, :, 4096:4160].rearrange("h p t -> p h t"), S["VSS"][sbi, 4096:4160, :], 64,
                       lambda kt: (Uadd[0:64, 0:64], Uadd)))
        fox(64, lambda h, p0=p0: qbTsv[:, h, p0:p0 + 64], chunks, ost)
        P.dma("sync", lambda e, sbi=sbi: e.dma_start(out=S["OT"][2048 + sbi * 64:2048 + sbi * 64 + 64, :], in_=ost[0][0:64, :]), reads=[ost[0].b])
        P.barrier()
        pp = psum_proj()


def phase_merge(k):
    k.tag = ""
    P, C, I, O, S = k.P, k.C, k.I, k.O, k.S
    hxT = get(k, "hxT", 8 * NTOK, BF16)
    combT = get(k, "combT", NTOK, BF16, parts=32)
    hxTv = hxT.v("p (k t) -> p k t", t=NTOK)
    Wg = get(k, "Wg", 8 * 2048, BF16)
    Wgv = Wg.v("p (k c) -> p k c", c=2048)
    wpa = get(k, "wpa", 4 * D, BF16)
    wpb = get(k, "wpb", 4 * D, BF16)
    wo = get(k, "wo", 8 * D, BF16)
    Wr = get(k, "Wr", 8 * 36)
    Wrv = Wr.v("p (k c) -> p k c", c=36)
    wpav, wpbv, wov = wpa.v("p (k c) -> p k c", c=D), wpb.v("p (k c) -> p k c", c=D), wo.v("p (k c) -> p k c", c=D)
    for kc in range(8):
        P.dma(G_, lambda e, kc=kc: e.dma_start(out=Wgv[:, kc, :], in_=I["w_in"][kc * 128:(kc + 1) * 128, C_G:C_G + 2048]), writes=[Wg.b])
        P.dma(G_, lambda e, kc=kc: e.dma_start(out=wov[:, kc, :], in_=I["w_o"][kc * 128:(kc + 1) * 128, :]), writes=[wo.b])
        P.dma("sync", lambda e, kc=kc: e.dma_start(out=Wrv[:, kc, 0:4], in_=I["w_rg"][kc * 128:(kc + 1) * 128, :]), writes=[Wr.b])
        for g in range(4):
            P.dma("sync", lambda e, kc=kc, g=g: e.dma_start(out=Wrv[:, kc, 4 + 8 * g:12 + 8 * g], in_=I["w_re"][g, kc * 128:(kc + 1) * 128, :]), writes=[Wr.b])
    for kc in range(4):
        P.dma(G_, lambda e, kc=kc: e.dma_start(out=wpav[:, kc, :], in_=I["w_pa"][kc * 128:(kc + 1) * 128, :]), writes=[wpa.b])
        P.dma(G_, lambda e, kc=kc: e.dma_start(out=wpbv[:, kc, :], in_=I["w_pb"][kc * 128:(kc + 1) * 128, :]), writes=[wpb.b])
    tr = pbank(k, "tr", 0, 1, BF16)
    gp = pbank(k, "gp", 1, 4)
    pa = pbank(k, "pa", 5, 2)
    sm = pbank(k, "sm", 7)
    xin = [get(k, "xin%d" % i, D) for i in range(2)]
    gT = get(k, "gT", 2048, BF16)
    OTt = get(k, "OTt", 1024, BF16)
    oT = get(k, "oT", 1024, BF16)
    ta = get(k, "ta", D)
    tb = get(k, "tb", D)
    uT = get(k, "uT", D, BF16)
    y1 = get(k, "y1", D)
    hxf = get(k, "hxf", D)
    hxTf = get(k, "hxTf", D)
    junk = get(k, "junk", D, BF16)
    ss, rs = get(k, "ss", 1), get(k, "rs", 1)
    sm_names = ["lg", "gmax", "ngmax", "ohg", "eg", "sume", "pg", "tmp32", "fsel", "m1", "oh1", "fm", "m2", "oh2", "dd", "e2", "den", "p1", "p2", "t8", "comb8", "comb"]
    sm_cols = [36, 1, 1, 4, 4, 1, 1, 32, 8, 1, 8, 8, 1, 8, 1, 1, 1, 1, 1, 8, 8, 32]
    R = {n: get(k, "r_" + n, c) for n, c in zip(sm_names, sm_cols)}
    ident, identf = C["ident"], C["identf"]
    for ti in range(17):
        src, r0 = (I["xown"], ti * 128) if ti < 16 else (I["xs"], 0)
        tok0 = ti * 128
        X = xin[ti % 2]
        P.dma("sync", lambda e, X=X, src=src, r0=r0: e.dma_start(out=X[:], in_=src[r0:r0 + 128, :]), writes=[X.b])
        P.dma("sync", lambda e, tok0=tok0: e.dma_start(out=OTt[:], in_=S["OT"][tok0:tok0 + 128, :]), writes=[OTt.b])
        HT = rms_tile(k, X, C["gmix"], tr)
        for gc in range(16):
            for kc in range(8):
                P.op(PE_, lambda e, gc=gc, kc=kc: e.matmul(gp[:, gc * 128:(gc + 1) * 128], lhsT=Wgv[:, kc, gc * 128:(gc + 1) * 128],
                                                           rhs=HT[:, kc * 128:(kc + 1) * 128], start=(kc == 0), stop=(kc == 7)),
                     reads=[Wg.b, HT.b], writes=[gp.b])
        P.op(S_, lambda e: e.activation(out=gT[:], in_=gp[:, 0:2048], func=AF.Sigmoid), reads=[gp.b], writes=[gT.b])
        for kk in range(8):
            P.op(PE_, lambda e, kk=kk: e.transpose(tr[:, kk * 128:(kk + 1) * 128], OTt[:, kk * 128:(kk + 1) * 128], ident[:]),
                 reads=[OTt.b, ident.b], writes=[tr.b])
        P.op(S_, lambda e: e.copy(out=oT[:], in_=tr[:, 0:1024]), reads=[tr.b], writes=[oT.b])
        for dc in range(8):
            for kk in range(4):
                P.op(PE_, lambda e, dc=dc, kk=kk: e.matmul(pa[:, dc * 128:(dc + 1) * 128], lhsT=wpav[:, kk, dc * 128:(dc + 1) * 128],
                                                           rhs=oT[:, kk * 128:(kk + 1) * 128], start=(kk == 0), stop=(kk == 3)),
                     reads=[wpa.b, oT.b], writes=[pa.b])
        for dc in range(8):
            for kk in range(4):
                P.op(PE_, lambda e, dc=dc, kk=kk: e.matmul(gp[:, 1024 + dc * 128:1024 + (dc + 1) * 128], lhsT=wpbv[:, kk, dc * 128:(dc + 1) * 128],
                                                           rhs=oT[:, (4 + kk) * 128:(5 + kk) * 128], start=(kk == 0), stop=(kk == 3)),
                     reads=[wpb.b, oT.b], writes=[gp.b])
        P.op(V_, lambda e: e.tensor_tensor(out=ta[:], in0=pa[:, 0:1024], in1=gT[:, 0:1024], op=ALU.mult), reads=[pa.b, gT.b], writes=[ta.b])
        P.op(V_, lambda e: e.tensor_tensor(out=tb[:], in0=gp[:, 1024:2048], in1=gT[:, 1024:2048], op=ALU.mult), reads=[gp.b, gT.b], writes=[tb.b])
        P.op(G_, lambda e: e.tensor_tensor(out=uT[:], in0=ta[:], in1=tb[:], op=ALU.add), reads=[ta.b, tb.b], writes=[uT.b])
        for half in range(2):
            for kk in range(8):
                P.op(PE_, lambda e, half=half, kk=kk: e.matmul(gp[:, half * 512:(half + 1) * 512], lhsT=uT[:, kk * 128:(kk + 1) * 128],
                                                               rhs=wov[:, kk, half * 512:(half + 1) * 512], start=(kk == 0), stop=(kk == 7)),
                     reads=[uT.b, wo.b], writes=[gp.b])
        P.op(V_, lambda e, X=X: e.tensor_tensor(out=y1[:], in0=gp[:, 0:1024], in1=X[:], op=ALU.add), reads=[gp.b, X.b], writes=[y1.b])
        P.dma("sync", lambda e, tok0=tok0: e.dma_start(out=S["Y1"][tok0:tok0 + 128, :], in_=y1[:]), reads=[y1.b])
        if os.environ.get("KNOROUTER"):
            P.dma(G_, lambda e, tok0=tok0: e.dma_start(out=O["o_dbg"][tok0:tok0 + 128, :], in_=OTt[:]), reads=[OTt.b])
            dsty = O["o_y"][tok0:tok0 + 128, :] if ti < 16 else O["o_ys"][:, :]
            P.dma("sync", lambda e, dsty=dsty: e.dma_start(out=dsty, in_=y1[:]), reads=[y1.b])
            continue
        P.op(S_, lambda e: e.activation(out=junk[:], in_=y1[:], func=AF.Square, accum_out=ss[:, 0:1]), reads=[y1.b], writes=[junk.b, ss.b])
        rsqrt_act(k, rs, ss, D)
        P.op(V_, lambda e: e.scalar_tensor_tensor(out=hxf[:], in0=y1[:], scalar=rs[:, 0:1], in1=C["gffn"][:], op0=ALU.mult, op1=ALU.mult),
             reads=[y1.b, rs.b, C["gffn"].b], writes=[hxf.b])
        for kc in range(8):
            P.op(PE_, lambda e, kc=kc: e.matmul(pa[:, kc * 128:(kc + 1) * 128], lhsT=hxf[:, kc * 128:(kc + 1) * 128], rhs=identf[:], start=True, stop=True),
                 reads=[hxf.b, identf.b], writes=[pa.b])
        P.op(S_, lambda e: e.copy(out=hxTf[:], in_=pa[:, 0:1024]), reads=[pa.b], writes=[hxTf.b])
        P.op(V_, lambda e, tok0=tok0: e.tensor_copy(out=hxTv[:, :, tok0:tok0 + 128], in_=hxTf.v("p (k t) -> p k t", t=128)),
             reads=[hxTf.b], writes=[hxT.b])
        for kc in range(8):
            P.op(PE_, lambda e, kc=kc: e.matmul(sm[:, 0:36], lhsT=hxTf[:, kc * 128:(kc + 1) * 128], rhs=Wrv[:, kc, :], start=(kc == 0), stop=(kc == 7)),
                 reads=[hxTf.b, Wr.b], writes=[sm.b])
        r = R
        def vo(fn, reads, writes):
            P.op(V_, fn, reads=[x.b for x in reads], writes=[x.b for x in writes])
        vo(lambda e: e.tensor_tensor(out=r["lg"][:, 0:4], in0=sm[:, 0:4], in1=C["brg"][:], op=ALU.add), [sm, C["brg"]], [r["lg"]])
        vo(lambda e: e.tensor_tensor(out=r["lg"][:, 4:36], in0=sm[:, 4:36], in1=C["bre"][:], op=ALU.add), [sm, C["bre"], r["lg"]], [r["lg"]])
        vo(lambda e: e.tensor_reduce(out=r["gmax"][:], in_=r["lg"][:, 0:4], axis=AX.X, op=ALU.max), [r["lg"]], [r["gmax"]])
        vo(lambda e: e.tensor_scalar(out=r["ohg"][:], in0=r["lg"][:, 0:4], scalar1=r["gmax"][:, 0:1], scalar2=None, op0=ALU.is_ge), [r["lg"], r["gmax"]], [r["ohg"]])
        vo(lambda e: e.tensor_scalar(out=r["ngmax"][:], in0=r["gmax"][:], scalar1=-1.0, scalar2=None, op0=ALU.mult), [r["gmax"]], [r["ngmax"]])
        P.op(S_, lambda e: e.activation(out=r["eg"][:], in_=r["lg"][:, 0:4], func=AF.Exp, bias=r["ngmax"][:, 0:1], scale=1.0, accum_out=r["sume"][:, 0:1]),
             reads=[r["lg"].b, r["ngmax"].b], writes=[r["eg"].b, r["sume"].b])
        vo(lambda e: e.reciprocal(out=r["pg"][:], in_=r["sume"][:]), [r["sume"]], [r["pg"]])
        vo(lambda e: e.tensor_tensor(out=r["tmp32"].v("p (g e) -> p g e", e=8), in0=r["lg"][:, 4:36].rearrange("p (g e) -> p g e", e=8),
                                     in1=r["ohg"][:].unsqueeze(2).to_broadcast([128, 4, 8]), op=ALU.mult), [r["lg"], r["ohg"]], [r["tmp32"]])
        vo(lambda e: e.tensor_reduce(out=r["fsel"][:], in_=r["tmp32"].v("p (g e) -> p e g", e=8), axis=AX.X, op=ALU.add), [r["tmp32"]], [r["fsel"]])
        vo(lambda e: e.tensor_reduce(out=r["m1"][:], in_=r["fsel"][:], axis=AX.X, op=ALU.max), [r["fsel"]], [r["m1"]])
        vo(lambda e: e.tensor_scalar(out=r["oh1"][:], in0=r["fsel"][:], scalar1=r["m1"][:, 0:1], scalar2=None, op0=ALU.is_ge), [r["fsel"], r["m1"]], [r["oh1"]])
        vo(lambda e: e.scalar_tensor_tensor(out=r["fm"][:], in0=r["oh1"][:], scalar=-1e30, in1=r["fsel"][:], op0=ALU.mult, op1=ALU.add), [r["oh1"], r["fsel"]], [r["fm"]])
        vo(lambda e: e.tensor_reduce(out=r["m2"][:], in_=r["fm"][:], axis=AX.X, op=ALU.max), [r["fm"]], [r["m2"]])
        vo(lambda e: e.tensor_scalar(out=r["oh2"][:], in0=r["fm"][:], scalar1=r["m2"][:, 0:1], scalar2=None, op0=ALU.is_ge), [r["fm"], r["m2"]], [r["oh2"]])
        vo(lambda e: e.tensor_sub(out=r["dd"][:], in0=r["m2"][:], in1=r["m1"][:]), [r["m2"], r["m1"]], [r["dd"]])
        P.op(S_, lambda e: e.activation(out=r["e2"][:], in_=r["dd"][:], func=AF.Exp), reads=[r["dd"].b], writes=[r["e2"].b])
        vo(lambda e: e.tensor_scalar(out=r["den"][:], in0=r["e2"][:], scalar1=1.0, scalar2=None, op0=ALU.add), [r["e2"]], [r["den"]])
        vo(lambda e: e.reciprocal(out=r["p1"][:], in_=r["den"][:]), [r["den"]], [r["p1"]])
        vo(lambda e: e.tensor_tensor(out=r["p2"][:], in0=r["e2"][:], in1=r["p1"][:], op=ALU.mult), [r["e2"], r["p1"]], [r["p2"]])
        vo(lambda e: e.tensor_scalar(out=r["t8"][:], in0=r["oh1"][:], scalar1=r["p1"][:, 0:1], scalar2=None, op0=ALU.mult), [r["oh1"], r["p1"]], [r["t8"]])
        vo(lambda e: e.scalar_tensor_tensor(out=r["comb8"][:], in0=r["oh2"][:], scalar=r["p2"][:, 0:1], in1=r["t8"][:], op0=ALU.mult, op1=ALU.add),
           [r["oh2"], r["p2"], r["t8"]], [r["comb8"]])
        vo(lambda e: e.tensor_scalar(out=r["comb8"][:], in0=r["comb8"][:], scalar1=r["pg"][:, 0:1], scalar2=None, op0=ALU.mult), [r["comb8"], r["pg"]], [r["comb8"]])
        vo(lambda e: e.tensor_tensor(out=r["comb"].v("p (g e) -> p g e", e=8), in0=r["ohg"][:].unsqueeze(2).to_broadcast([128, 4, 8]),
                                     in1=r["comb8"][:].unsqueeze(1).to_broadcast([128, 4, 8]), op=ALU.mult), [r["ohg"], r["comb8"]], [r["comb"]])
        P.op(PE_, lambda e: e.matmul(sm[0:32, 64:192], lhsT=r["comb"][:], rhs=identf[:], start=True, stop=True), reads=[r["comb"].b, identf.b], writes=[sm.b])
        P.op(V_, lambda e, tok0=tok0: e.tensor_copy(out=combT[:, tok0:tok0 + 128], in_=sm[0:32, 64:192]), reads=[sm.b], writes=[combT.b])


def phase_moe(k):
    k.tag = ""
    P, C, I, O, S = k.P, k.C, k.I, k.O, k.S
    hxT = get(k, "hxT", 8 * NTOK, BF16)
    combT = get(k, "combT", NTOK, BF16, parts=32)
    hxTv = hxT.v("p (k t) -> p k t", t=NTOK)
    yacc = get(k, "yacc", 17 * D)
    yv = yacc.v("p (a d) -> p a d", d=D)
    for ti in range(17):
        P.dma("sync", lambda e, ti=ti: e.dma_start(out=yv[:, ti, :], in_=S["Y1"][ti * 128:(ti + 1) * 128, :]), writes=[yacc.b])
    Sel = get(k, "Sel", 32 * 128, BF16, parts=32)
    Selv = Sel.v("p (e c) -> p e c", c=128)
    P.op(G_, lambda e: e.memset(Sel[:], 0.0), writes=[Sel.b])
    P.op(G_, lambda e: e.affine_select(out=Selv, in_=Selv, pattern=[[-1, 32], [0, 128]], compare_op=ALU.not_equal, fill=1.0,
                                       base=0, channel_multiplier=1), reads=[Sel.b], writes=[Sel.b])
    W13 = [get(k, "W13_%d" % i, 2 * 8 * 256, BF16) for i in range(2)]
    W2 = [get(k, "W2_%d" % i, 2 * D, BF16) for i in range(2)]
    s1 = [get(k, "s1_%d" % i, 256) for i in range(2)]
    tt_ = [get(k, "tt_%d" % i, 256) for i in range(2)]
    hid = [get(k, "hid%d" % i, 256, BF16) for i in range(2)]
    h13 = [pbank(k, "h13_%d" % i, i) for i in range(2)]
    cbb = pbank(k, "cbb", 2)
    yp = [pbank(k, "yp%d" % i, 3 + 2 * i, 2) for i in range(2)]

    def load_w(gi):
        b = gi % 2
        Wv = W13[b].v("p (e k c) -> p e k c", k=8, c=256)
        W2v = W2[b].v("p (e d) -> p e d", d=D)
        for el in range(2):
            e_ = 2 * gi + el
            P.dma(G_, lambda e, el=el, e_=e_, Wv=Wv: e.dma_start(out=Wv[:, el, :, 0:128], in_=I["w1"][e_].rearrange("(k p) f -> p k f", p=128)), writes=[W13[b].b])
            P.dma(G_, lambda e, el=el, e_=e_, Wv=Wv: e.dma_start(out=Wv[:, el, :, 128:256], in_=I["w3"][e_].rearrange("(k p) f -> p k f", p=128)), writes=[W13[b].b])
            P.dma(G_, lambda e, el=el, e_=e_, W2v=W2v: e.dma_start(out=W2v[:, el, :], in_=I["w2"][e_]), writes=[W2[b].b])

    blocks = [(i * 256, 256) for i in range(8)] + [(2048, 128)]
    load_w(0)

    def down(item):
        (tok0, nt, el, HID, W2v, b) = item
        for tt in range(nt // 128):
            for half in range(2):
                P.op(PE_, lambda e, tt=tt, half=half: e.matmul(yp[tt][:, half * 512:(half + 1) * 512], lhsT=HID[:, tt * 128:(tt + 1) * 128],
                                                               rhs=W2v[:, el, half * 512:(half + 1) * 512], start=(el == 0), stop=(el == 1)),
                     reads=[HID.b, W2[b].b], writes=[yp[tt].b])
        if el == 1:
            for tt in range(nt // 128):
                ti = tok0 // 128 + tt
                P.op(V_, lambda e, tt=tt, ti=ti: e.tensor_tensor(out=yv[:, ti, :], in0=yp[tt][:, 0:1024], in1=yv[:, ti, :], op=ALU.add),
                     reads=[yp[tt].b, yacc.b], writes=[yacc.b])

    def up(u, tok0, nt, el, e_, Wv, b):
        H, S1, TT, HID = h13[u % 2], s1[u % 2], tt_[u % 2], hid[u % 2]
        cb = cbb[:, (u % 2) * 256:(u % 2) * 256 + nt]
        for (c0, o0) in ((0, 0), (128, 256)):
            for kc in range(8):
                P.op(PE_, lambda e, c0=c0, o0=o0, kc=kc: e.matmul(H[:, o0:o0 + nt], lhsT=Wv[:, el, kc, c0:c0 + 128], rhs=hxTv[:, kc, tok0:tok0 + nt],
                                                                  start=(kc == 0), stop=(kc == 7)), reads=[W13[b].b, hxT.b], writes=[H.b])
        P.op(PE_, lambda e: e.matmul(cb, lhsT=Selv[:, e_, :], rhs=combT[:, tok0:tok0 + nt], start=True, stop=True),
             reads=[Sel.b, combT.b], writes=[cbb.b])
        P.op(S_, lambda e: e.activation(out=S1[:, 0:nt], in_=H[:, 0:nt], func=AF.Silu), reads=[H.b], writes=[S1.b])
        P.op(V_, lambda e: e.tensor_tensor(out=TT[:, 0:nt], in0=H[:, 256:256 + nt], in1=S1[:, 0:nt], op=ALU.mult),
             reads=[H.b, S1.b], writes=[TT.b])
        P.op(V_, lambda e: e.tensor_tensor(out=HID[:, 0:nt], in0=cb, in1=TT[:, 0:nt], op=ALU.mult),
             reads=[cbb.b, TT.b], writes=[HID.b])
        return HID

    u = 0
    for gi in range(16):
        if gi + 1 < 16:
            load_w(gi + 1)
        b = gi % 2
        Wv = W13[b].v("p (e k c) -> p e k c", k=8, c=256)
        W2v = W2[b].v("p (e d) -> p e d", d=D)
        pend = []
        for (tok0, nt) in blocks:
            for el in range(2):
                HID = up(u, tok0, nt, el, 2 * gi + el, Wv, b)
                u += 1
                if pend:
                    down(pend.pop(0))
                pend.append((tok0, nt, el, HID, W2v, b))
        while pend:
            down(pend.pop(0))
    for ti in range(17):
        dst = O["o_y"][ti * 128:(ti + 1) * 128, :] if ti < 16 else O["o_ys"][:, :]
        P.dma("sync", lambda e, ti=ti, dst=dst: e.dma_start(out=dst, in_=yv[:, ti, :]), reads=[yacc.b])


_CACHE = {}


def kernel(**inp):
    f = lambda a: np.ascontiguousarray(np.asarray(a, dtype=np.float32))
    x_prompt = f(inp["x_prompt"])
    x_sample = f(inp["x_sample"])
    if "nc" not in _CACHE:
        _CACHE["nc"] = build_program()
    nc = _CACHE["nc"]
    cak, cav = f(inp["cache_a_k"])[0].reshape(16, 512, 512), f(inp["cache_a_v"])[0].reshape(16, 512, 512)
    cbk, cbv = f(inp["cache_b_k"])[0].reshape(16, 4096, 512), f(inp["cache_b_v"])[0].reshape(16, 4096, 512)
    cblf = f(inp["cache_b_logf"])[0]
    shared = {
        "g_mix": f(inp["g_mix"]), "w_in": f(inp["w_in"])[0], "b_f": f(inp["b_f"]),
        "q_norm_a": f(inp["q_norm_a"]), "k_norm_a": f(inp["k_norm_a"]), "q_norm_b": f(inp["q_norm_b"]), "k_norm_b": f(inp["k_norm_b"]),
        "rel_bias": f(inp["rel_bias"])[0], "w_pa": f(inp["w_pa"])[0], "w_pb": f(inp["w_pb"])[0], "w_o": f(inp["w_o"])[0],
        "g_ffn": f(inp["g_ffn"]), "w_rg": f(inp["w_rg"])[0], "b_rg": f(inp["b_rg"]), "w_re": f(inp["w_re"])[0],
        "b_re": f(inp["b_re"]).reshape(1, 32), "w1": f(inp["w1"])[0], "w3": f(inp["w3"])[0], "w2": f(inp["w2"])[0],
    }
    in_maps = []
    for c in range(8):
        b, j = c // 4, c % 4
        blocks = [4 * n + j for n in range(4)]
        xown = np.concatenate([x_prompt[b, g * 512:(g + 1) * 512] for g in blocks], axis=0)
        xhalo = np.concatenate([x_prompt[b, (g - 1) * 512:g * 512] if g > 0 else np.zeros((512, D), np.float32) for g in blocks], axis=0)
        wm = np.zeros((64, 4), np.float32)
        hv = np.ones((1, 4), np.float32)
        for n in range(4):
            wm[:4 * blocks[n], n] = 1.0
            if blocks[n] == 0:
                hv[0, n] = 0.0
        m = dict(shared)
        m.update({
            "xfull": x_prompt[b], "xown": xown, "xhalo": xhalo, "xs": x_sample[2 * c:2 * c + 2].reshape(128, D),
            "cak": cak[2 * c:2 * c + 2], "cav": cav[2 * c:2 * c + 2], "cbk": cbk[2 * c:2 * c + 2], "cbv": cbv[2 * c:2 * c + 2],
            "cblf": cblf[2 * c:2 * c + 2],
            "wmeta": wm.reshape(1, 256), "hvalid": hv, "qpos": (j * 512 + np.arange(512, dtype=np.float32)).reshape(1, 512),
        })
        in_maps.append(m)
    res = run_bass_kernel_spmd(nc, in_maps, core_ids=list(range(8)))
    R = res.results
    _CACHE["last"] = R
    y_p = np.zeros((2, SEQ, D), np.float32)
    for c in range(8):
        b, j = c // 4, c % 4
        for n in range(4):
            g = 4 * n + j
            y_p[b, g * 512:(g + 1) * 512] = R[c]["o_y"][n * 512:(n + 1) * 512]
    y_s = np.concatenate([R[c]["o_ys"].reshape(2, 64, D) for c in range(8)], axis=0)
    a_k_p = np.stack([R[3]["o_ak"], R[7]["o_ak"]]).reshape(1, 2, 512, NH, HD)
    a_v_p = np.stack([R[3]["o_av"], R[7]["o_av"]]).reshape(1, 2, 512, NH, HD)
    b_k_p = np.stack([R[0]["o_bk"], R[4]["o_bk"]]).reshape(1, 2, SEQ, NH, HD)
    b_v_p = np.stack([R[0]["o_bv"], R[4]["o_bv"]]).reshape(1, 2, SEQ, NH, HD)
    b_lf_p = np.stack([R[0]["o_blf"], R[4]["o_blf"]]).reshape(1, 2, SEQ, NH)
    a_k_s = np.concatenate([R[c]["o_aks"] for c in range(8)], axis=0).reshape(1, 16, 512, NH, HD)
    a_v_s = np.concatenate([R[c]["o_avs"] for c in range(8)], axis=0).reshape(1, 16, 512, NH, HD)
    b_k_s = np.concatenate([R[c]["o_bks"].reshape(2, 64, 512) for c in range(8)], axis=0).reshape(1, 16, 64, NH, HD)
    b_v_s = np.concatenate([R[c]["o_bvs"].reshape(2, 64, 512) for c in range(8)], axis=0).reshape(1, 16, 64, NH, HD)
    b_lf_s = np.concatenate([R[c]["o_blfs"].reshape(2, 64, NH) for c in range(8)], axis=0).reshape(1, 16, 64, NH)
    return (y_p, y_s, a_k_p, a_v_p, b_k_p, b_v_p, b_lf_p, a_k_s, a_v_s, b_k_s, b_v_s, b_lf_s)
```
